# Optimizing a Trainium2 kernel written in Bass

```python
import math
import jax, jax.numpy as jnp
from jax import lax
import numpy as np

D_MODEL = 4096
BATCH = 1
SEQ = 8192
DEPTH = 1

POOL_GROUPS = 4
POOL_GROUP_WIDTH = 512
POOL_WIDTH = POOL_GROUPS * POOL_GROUP_WIDTH
POOL_WINDOWS = (2, 4, 8, 16)
N_HEADS = 16
N_KV_GROUPS = 4
HEADS_PER_GROUP = N_HEADS // N_KV_GROUPS
HEAD_DIM = 128
Q_WIDTH = N_HEADS * HEAD_DIM
KV_WIDTH = N_KV_GROUPS * HEAD_DIM
CMP_BLOCK = 32
CMP_STRIDE = 16
SLC_BLOCK = 64
SLC_TOPK = 16
WINDOW = 512
Q_BLOCK = 128
FORCE_SCORE = 1e4
PEER_HEADS = 8
PEER_NKEYS = 128
PEER_N_EXPERTS = PEER_NKEYS * PEER_NKEYS
PEER_QDIM = 256
PEER_HALF = PEER_QDIM // 2
PEER_TOPK = 16
PEER_TOKEN_BLOCK = 128
PLE_DIM = 256
RMS_EPS = 1e-6
NEG = -1e30
IN_SPLITS = (POOL_WIDTH, Q_WIDTH, 6 * KV_WIDTH, 3 * N_HEADS, D_MODEL, D_MODEL)
IN_WIDTH = sum(IN_SPLITS)

kernel_name = "hybrid_pool_nsa_peer_block"


def rms_norm(x, g):
    xf = x.astype(jnp.float32)
    y = xf * lax.rsqrt(jnp.mean(xf * xf, axis=-1, keepdims=True) + RMS_EPS)
    return (y * g.astype(jnp.float32)).astype(x.dtype)


def alibi_slopes():
    s = 2.0 ** (-8.0 * np.arange(1, N_HEADS + 1) / N_HEADS)
    return jnp.asarray(s, dtype=jnp.float32).reshape(N_KV_GROUPS, HEADS_PER_GROUP)


def pool_mixer(u, pool_w, pool_scale):
    B, T, _ = u.shape
    ug = u.astype(jnp.float32).reshape(B, T, POOL_GROUPS, POOL_GROUP_WIDTH)
    cs = jnp.cumsum(ug, axis=1)
    pos = jnp.arange(T, dtype=jnp.float32)
    outs = []
    for gi, w in enumerate(POOL_WINDOWS):
        c = cs[:, :, gi]
        lower = jnp.pad(c, ((0, 0), (w, 0), (0, 0)))[:, :T]
        cnt = jnp.minimum(pos + 1.0, float(w))[None, :, None]
        outs.append((c - lower) / cnt)
    pooled = jnp.stack(outs, axis=2) - ug
    mixed = jnp.einsum('btgc,gcd->btgd', pooled.astype(u.dtype), pool_w)
    return mixed.reshape(B, T, POOL_WIDTH) * pool_scale


def compress_blocks(kv, pos_emb, w):
    T = kv.shape[1]
    n_cmp = (T - CMP_BLOCK) // CMP_STRIDE + 1
    idx = (np.arange(n_cmp)[:, None] * CMP_STRIDE + np.arange(CMP_BLOCK)[None, :]).astype(np.int32)
    blocks = kv[:, idx] + pos_emb[None, None, :, None, :]
    return jnp.einsum('bnlgd,ldc->bngc', blocks, w.reshape(CMP_BLOCK, HEAD_DIM, HEAD_DIM))


def nsa_attention(q, k_cmp, v_cmp, k_slc, v_slc, k_win, v_win, gates,
                  cmp_pos_k, cmp_pos_v, cmp_w_k, cmp_w_v):
    B, T = q.shape[:2]
    G, Hg, dk = N_KV_GROUPS, HEADS_PER_GROUP, HEAD_DIM
    slopes = alibi_slopes()
    qh = (q.reshape(B, T, G, Hg, dk) * (dk ** -0.5)).transpose(0, 2, 3, 1, 4)
    tpos = jnp.arange(T, dtype=jnp.int32)

    kc = compress_blocks(k_cmp, cmp_pos_k, cmp_w_k)
    vc = compress_blocks(v_cmp, cmp_pos_v, cmp_w_v)
    n_cmp = kc.shape[1]
    cmp_end = jnp.arange(n_cmp, dtype=jnp.int32) * CMP_STRIDE + CMP_BLOCK - 1
    dist_c = (tpos[:, None] - cmp_end[None, :]).astype(jnp.float32)
    valid_c = dist_c >= 0
    s_c = jnp.einsum('bghtd,bngd->bghtn', qh, kc).astype(jnp.float32) \
        - slopes[None, :, :, None, None] * dist_c
    s_c = jnp.where(valid_c, s_c, NEG)
    p_c = jax.nn.softmax(s_c, axis=-1) * valid_c
    o_cmp = jnp.einsum('bghtn,bngd->bghtd', p_c.astype(vc.dtype), vc)

    n_sel = T // SLC_BLOCK
    k_sel = min(SLC_TOPK, n_sel)
    c_start = np.arange(n_cmp) * CMP_STRIDE
    s_start = np.arange(n_sel) * SLC_BLOCK
    overlap = ((c_start[:, None] < s_start[None, :] + SLC_BLOCK) &
               (c_start[:, None] + CMP_BLOCK > s_start[None, :])).astype(np.float32)
    imp = jnp.einsum('bghtn,nk->bgtk', p_c, jnp.asarray(overlap))
    cur = (tpos // SLC_BLOCK)[:, None]
    kk = jnp.arange(n_sel, dtype=jnp.int32)[None, :]
    forced = (kk == 0) | (kk == cur) | (kk == cur - 1)
    score = jnp.where(forced, FORCE_SCORE, jnp.where(kk <= cur, imp, -1.0))
    _, sel_idx = lax.top_k(score, k_sel)
    sel_ok = sel_idx <= cur[None, None]

    n_qb = T // Q_BLOCK
    ks = k_slc.reshape(B, n_sel, SLC_BLOCK, G, dk).transpose(0, 3, 1, 2, 4)
    vs = v_slc.reshape(B, n_sel, SLC_BLOCK, G, dk).transpose(0, 3, 1, 2, 4)
    q_b = jnp.moveaxis(qh.reshape(B, G, Hg, n_qb, Q_BLOCK, dk), 3, 0)
    idx_b = jnp.moveaxis(sel_idx.reshape(B, G, n_qb, Q_BLOCK, k_sel), 2, 0)
    ok_b = jnp.moveaxis(sel_ok.reshape(B, G, n_qb, Q_BLOCK, k_sel), 2, 0)
    t_b = tpos.reshape(n_qb, Q_BLOCK)
    bi = jnp.arange(B)[:, None, None, None]
    gi = jnp.arange(G)[None, :, None, None]
    in_blk = jnp.arange(SLC_BLOCK, dtype=jnp.int32)

    def sel_block(args):
        qb, ib, okb, tb = args
        kg = ks[bi, gi, ib]
        vg = vs[bi, gi, ib]
        spos = ib[..., None] * SLC_BLOCK + in_blk
        d = tb[None, None, :, None, None] - spos
        m = okb[..., None] & (d >= 0)
        sc = jnp.einsum('bghqd,bgqkld->bghqkl', qb, kg).astype(jnp.float32) \
            - slopes[None, :, :, None, None, None] * d[:, :, None].astype(jnp.float32)
        sc = jnp.where(m[:, :, None], sc, NEG)
        sh = sc.shape
        pr = jax.nn.softmax(sc.reshape(sh[:4] + (-1,)), axis=-1).reshape(sh)
        return jnp.einsum('bghqkl,bgqkld->bghqd', pr.astype(vg.dtype), vg)

    o_slc = lax.map(sel_block, (q_b, idx_b, ok_b, t_b))
    o_slc = jnp.moveaxis(o_slc, 0, 3).reshape(B, G, Hg, T, dk)

    span = WINDOW + Q_BLOCK
    kp = jnp.pad(k_win, ((0, 0), (WINDOW, 0), (0, 0), (0, 0)))
    vp = jnp.pad(v_win, ((0, 0), (WINDOW, 0), (0, 0), (0, 0)))
    widx = (np.arange(n_qb)[:, None] * Q_BLOCK + np.arange(span)[None, :]).astype(np.int32)
    kw = kp[:, widx]
    vw = vp[:, widx]
    spos_w = jnp.asarray(widx - WINDOW)
    d_w = t_b[:, :, None] - spos_w[:, None, :]
    m_w = (d_w >= 0) & (d_w < WINDOW) & (spos_w[:, None, :] >= 0)
    qw = qh.reshape(B, G, Hg, n_qb, Q_BLOCK, dk)
    s_w = jnp.einsum('bghiqd,bisgd->bghiqs', qw, kw).astype(jnp.float32) \
        - slopes[None, :, :, None, None, None] * d_w.astype(jnp.float32)
    s_w = jnp.where(m_w, s_w, NEG)
    p_w = jax.nn.softmax(s_w, axis=-1)
    o_win = jnp.einsum('bghiqs,bisgd->bghiqd', p_w.astype(vw.dtype), vw).reshape(B, G, Hg, T, dk)

    gt = jax.nn.sigmoid(gates.astype(jnp.float32)).reshape(B, T, G, Hg, 3).transpose(0, 2, 3, 1, 4)
    o = gt[..., 0:1] * o_cmp + gt[..., 1:2] * o_slc + gt[..., 2:3] * o_win
    return o.transpose(0, 3, 1, 2, 4).reshape(B, T, Q_WIDTH).astype(q.dtype)


def peer_ffn(h, w_q, keys1, keys2, u_tab, v_tab):
    B, T, D = h.shape
    q = (h @ w_q).reshape(B, T, PEER_HEADS, PEER_QDIM)
    s1 = jnp.einsum('bthd,nd->bthn', q[..., :PEER_HALF], keys1).astype(jnp.float32)
    s2 = jnp.einsum('bthd,nd->bthn', q[..., PEER_HALF:], keys2).astype(jnp.float32)
    v1, i1 = lax.top_k(s1, PEER_TOPK)
    v2, i2 = lax.top_k(s2, PEER_TOPK)
    cand = (v1[..., :, None] + v2[..., None, :]).reshape(B, T, PEER_HEADS, PEER_TOPK * PEER_TOPK)
    vbest, ci = lax.top_k(cand, PEER_TOPK)
    e = jnp.take_along_axis(i1, ci // PEER_TOPK, axis=-1) * PEER_NKEYS \
        + jnp.take_along_axis(i2, ci % PEER_TOPK, axis=-1)
    g = jax.nn.softmax(vbest, axis=-1)
    n_tb = (B * T) // PEER_TOKEN_BLOCK
    n_sel = PEER_HEADS * PEER_TOPK
    xb = h.reshape(n_tb, PEER_TOKEN_BLOCK, D)
    eb = e.reshape(n_tb, PEER_TOKEN_BLOCK, n_sel)
    gb = g.reshape(n_tb, PEER_TOKEN_BLOCK, n_sel)

    def expert_block(args):
        xt, et, gt = args
        a = jnp.einsum('nd,nkd->nk', xt, u_tab[et]).astype(jnp.float32)
        coef = gt * jax.nn.gelu(a)
        return jnp.einsum('nk,nkd->nd', coef.astype(v_tab.dtype), v_tab[et])

    out = lax.map(expert_block, (xb, eb, gb))
    return out.reshape(B, T, D).astype(h.dtype)


def setup_inputs(seed: int = 0) -> dict:
    key = jax.random.key(seed)
    ks = jax.random.split(key, 24)
    f32 = jnp.float32
    L, D = DEPTH, D_MODEL

    def nrm(k, shape, scale):
        return jax.random.normal(k, shape, f32) * scale

    def gain(k, shape):
        return 1.0 + 0.02 * jax.random.normal(k, shape, f32)

    return {
        "x": nrm(ks[0], (BATCH, SEQ, D), 1.0),
        "p": nrm(ks[1], (DEPTH, BATCH, SEQ, PLE_DIM), 1.0),
        "norm_mix_g": gain(ks[2], (L, D)),
        "w_in": nrm(ks[3], (L, D, IN_WIDTH), D ** -0.5),
        "pool_w": nrm(ks[4], (L, POOL_GROUPS, POOL_GROUP_WIDTH, POOL_GROUP_WIDTH), POOL_GROUP_WIDTH ** -0.5),
        "pool_scale": gain(ks[5], (L, POOL_WIDTH)),
        "cmp_pos_k": nrm(ks[6], (L, CMP_BLOCK, HEAD_DIM), 0.02),
        "cmp_pos_v": nrm(ks[7], (L, CMP_BLOCK, HEAD_DIM), 0.02),
        "cmp_w_k": nrm(ks[8], (L, CMP_BLOCK * HEAD_DIM, HEAD_DIM), (CMP_BLOCK * HEAD_DIM) ** -0.5),
        "cmp_w_v": nrm(ks[9], (L, CMP_BLOCK * HEAD_DIM, HEAD_DIM), (CMP_BLOCK * HEAD_DIM) ** -0.5),
        "w_up_pool": nrm(ks[10], (L, POOL_WIDTH, D), POOL_WIDTH ** -0.5),
        "w_up_nsa": nrm(ks[11], (L, Q_WIDTH, D), Q_WIDTH ** -0.5),
        "w_out": nrm(ks[12], (L, D, D), D ** -0.5),
        "norm_ffn_g": gain(ks[13], (L, D)),
        "peer_w_q": nrm(ks[14], (L, D, PEER_HEADS * PEER_QDIM), D ** -0.5),
        "peer_keys1": nrm(ks[15], (L, PEER_NKEYS, PEER_HALF), PEER_HALF ** -0.5),
        "peer_keys2": nrm(ks[16], (L, PEER_NKEYS, PEER_HALF), PEER_HALF ** -0.5),
        "peer_u": nrm(ks[17], (L, PEER_N_EXPERTS, D), D ** -0.5),
        "peer_v": nrm(ks[18], (L, PEER_N_EXPERTS, D), PEER_HEADS ** -0.5),
        "norm_ple_g": gain(ks[19], (L, D)),
        "ple_w_gate": nrm(ks[20], (L, D, D), D ** -0.5),
        "ple_w_proj": nrm(ks[21], (L, PLE_DIM, D), PLE_DIM ** -0.5),
        "norm_final_g": gain(ks[22], (D,)),
    }


def reference(x, p, norm_mix_g, w_in, pool_w, pool_scale, cmp_pos_k, cmp_pos_v, cmp_w_k, cmp_w_v,
              w_up_pool, w_up_nsa, w_out, norm_ffn_g, peer_w_q, peer_keys1, peer_keys2, peer_u, peer_v,
              norm_ple_g, ple_w_gate, ple_w_proj, norm_final_g):
    B, T, _ = x.shape
    offsets = [int(o) for o in np.cumsum(IN_SPLITS)[:-1]]
    for i in range(DEPTH):
        h = rms_norm(x, norm_mix_g[i])
        z = h @ w_in[i]
        u_pool, q, kv, nsa_gates, gate_pool, gate_nsa = jnp.split(z, offsets, axis=-1)
        kv = kv.reshape(B, T, 6, N_KV_GROUPS, HEAD_DIM)
        pool_out = pool_mixer(u_pool, pool_w[i], pool_scale[i])
        nsa_out = nsa_attention(q, kv[:, :, 0], kv[:, :, 1], kv[:, :, 2], kv[:, :, 3], kv[:, :, 4], kv[:, :, 5],
                                nsa_gates, cmp_pos_k[i], cmp_pos_v[i], cmp_w_k[i], cmp_w_v[i])
        merged = jax.nn.sigmoid(gate_pool) * (pool_out.astype(x.dtype) @ w_up_pool[i]) \
            + jax.nn.sigmoid(gate_nsa) * (nsa_out @ w_up_nsa[i])
        x = x + merged.astype(x.dtype) @ w_out[i]
        h2 = rms_norm(x, norm_ffn_g[i])
        x = x + peer_ffn(h2, peer_w_q[i], peer_keys1[i], peer_keys2[i], peer_u[i], peer_v[i])
        r = rms_norm(x, norm_ple_g[i])
        x = x + (jax.nn.sigmoid(r @ ple_w_gate[i]) * (p[i] @ ple_w_proj[i])).astype(x.dtype)
    return rms_norm(x, norm_final_g)
```

```python
import math
from contextlib import ExitStack

import numpy as np
import ml_dtypes

import concourse.bass as bass
import concourse.mybir as mybir
from concourse.bass_utils import run_bass_kernel_spmd

F32 = mybir.dt.float32
BF16 = mybir.dt.bfloat16
AF = mybir.ActivationFunctionType
ALU = mybir.AluOpType
NPBF = ml_dtypes.bfloat16

NCORES = 8
T = 8192
D = 4096
NT = 1024
NH = 512
NE = NT + NH
INW = 15408
EPS = 1e-6
BIG = 32768.0
POOL_WINDOWS = (2, 4, 8, 16)
SLOPES = (2.0 ** (-8.0 * np.arange(1, 17) / 16)).astype(np.float32)
SAME_ENGINE_SYNC = True


class Buf:
    __slots__ = ("w", "r")

    def __init__(self):
        self.w = {}
        self.r = {}


class Tile:
    def __init__(self, kb, name, shape, dtype, space="sb", stack=None):
        nc = kb.nc
        cm = nc.sbuf_tensor(name, shape, dtype) if space == "sb" else nc.psum_tensor(name, shape, dtype)
        self.t = (stack if stack is not None else kb.stack).enter_context(cm)
        self.b = Buf()


class KB:
    ENG = ("pe", "act", "dve", "pool", "sp")

    def __init__(self):
        self.nc = bass.Bass("TRN2", target_bir_lowering=False)
        nc = self.nc
        self.gstack = ExitStack()
        self.stack = self.gstack
        self.e = {"pe": nc.tensor, "act": nc.scalar, "dve": nc.vector, "pool": nc.gpsimd, "sp": nc.sync}
        self.semh = {}
        self.cnt = {}
        for en in self.ENG:
            self.semh["e_" + en] = self.gstack.enter_context(nc.semaphore("sem_" + en))
            self.cnt["e_" + en] = 0
        self.ND = 40
        for i in range(self.ND):
            self.semh["d_%d" % i] = self.gstack.enter_context(nc.semaphore("semd_%d" % i))
            self.cnt["d_%d" % i] = 0
        self.dma_rr = 0
        self.seen = {en: {} for en in self.ENG}
        self.uid = 0
        self.ps = [Tile(self, "psb%d" % i, [128, 512], F32, "ps", self.gstack) for i in range(8)]
        self.rr = {}

    def name(self, s):
        self.uid += 1
        return "%s_%d" % (s, self.uid)

    def tile(self, name, shape, dtype):
        return Tile(self, self.name(name), shape, dtype, "sb")

    def rot(self, key, lst):
        i = self.rr.get(key, 0)
        self.rr[key] = i + 1
        return lst[i % len(lst)]

    def _waits(self, eng, w, r):
        waits = {}
        for b in r:
            for s, v in b.w.items():
                if waits.get(s, 0) < v:
                    waits[s] = v
        for b in w:
            for dct in (b.w, b.r):
                for s, v in dct.items():
                    if waits.get(s, 0) < v:
                        waits[s] = v
        own = "e_" + eng
        E = self.e[eng]
        seen = self.seen[eng]
        for s, v in waits.items():
            if s == own and (eng == "pe" or not SAME_ENGINE_SYNC):
                continue
            if seen.get(s, 0) >= v:
                continue
            E.wait_ge(self.semh[s], v)
            seen[s] = v

    def _record(self, tag, w, r):
        s, v = tag
        for b in r:
            if b.r.get(s, 0) < v:
                b.r[s] = v
        for b in w:
            b.w = {s: v}
            b.r = {}

    def op(self, eng, fn, w=(), r=()):
        self._waits(eng, w, r)
        inst = fn()
        s = "e_" + eng
        self.cnt[s] += 1
        inst.then_inc(self.semh[s], 1)
        self._record((s, self.cnt[s]), w, r)

    def dma(self, q, pairs, w=(), r=()):
        s = "d_%d" % (self.dma_rr % self.ND)
        self.dma_rr += 1
        E = self.e[q]
        seen = self.seen[q]
        prev = self.cnt[s]
        if prev > 0 and seen.get(s, 0) < prev:
            E.wait_ge(self.semh[s], prev)
            seen[s] = prev
        self._waits(q, w, r)
        for (o, i) in pairs:
            E.dma_start(out=o, in_=i).then_inc(self.semh[s], 16)
            self.cnt[s] += 16
        self._record((s, self.cnt[s]), w, r)

    def barrier(self):
        for en in self.ENG:
            E = self.e[en]
            seen = self.seen[en]
            for s, v in self.cnt.items():
                if v == 0 or s == "e_" + en:
                    continue
                if seen.get(s, 0) >= v:
                    continue
                E.wait_ge(self.semh[s], v)
                seen[s] = v

    def push(self):
        self._saved = self.stack
        self.stack = ExitStack()

    def pop(self):
        self.barrier()
        self.stack.close()
        self.stack = self._saved

    def phase(self):
        self.barrier()
        if self.stack is not self.gstack:
            self.stack.close()
        self.stack = ExitStack()
        self.rr = {}


def hml(v):
    v = np.asarray(v, np.float32)
    hi = v.astype(NPBF)
    r1 = v - hi.astype(np.float32)
    mid = r1.astype(NPBF)
    r2 = r1 - mid.astype(np.float32)
    lo = r2.astype(NPBF)
    return np.stack([hi, mid, lo], 0)


def static_tables():
    k = np.arange(128)
    tb = {}
    tb["ident_f"] = np.eye(128, dtype=np.float32)
    tb["ident_b"] = np.eye(128, dtype=np.float32).astype(NPBF)
    tb["ones3"] = np.ones((3, 128), np.float32).astype(NPBF)
    j = np.arange(512, dtype=np.float32)
    aq = np.zeros((3, 16, 512), NPBF)
    for h in range(16):
        aq[:, h, :] = hml(-SLOPES[h] * j)
    tb["aq"] = aq
    n = np.arange(512)
    c_start = n * 16
    s_start = np.arange(128) * 64
    ov = ((c_start[:, None] < s_start[None, :] + 64) & (c_start[:, None] + 32 > s_start[None, :])).astype(np.float32)
    ov[511, :] = 0.0
    rc = np.zeros((512, 129), np.float32)
    rc[:, :128] = ov
    rc[:511, 128] = 1.0
    tb["ovl1"] = rc.reshape(4, 128, 129).transpose(1, 0, 2).astype(NPBF).copy()
    eh = np.zeros((128, 56, 128), np.float32)
    for jj in range(56):
        eh[2 * jj, jj, :64] = 1.0
        eh[2 * jj + 1, jj, 64:] = 1.0
    eh[112:115, :, :] = 1.0
    tb["ehx"] = eh.astype(NPBF)
    eo = np.zeros((19, 8, 128), np.float32)
    for jj in range(8):
        eo[2 * jj, jj, :64] = 1.0
        eo[2 * jj + 1, jj, 64:] = 1.0
    eo[16:19, :, :] = 1.0
    tb["eox"] = eo.astype(NPBF)
    q = np.arange(512)
    cd = np.zeros((128, 4, 512), np.float32)
    for r in range(4):
        cd[:, r, :] = np.where((128 * r + k)[:, None] > q[None, :], -BIG, 0.0)
    tb["cdiag"] = cd.astype(NPBF)
    kbo = np.zeros((128, 16, 2, 8), np.float32)
    for h in range(16):
        for qt in range(2):
            for jo in range(8):
                kbo[:, h, qt, jo] = SLOPES[h] * (128 * jo + k - 512 * qt)
    tb["kb_o"] = kbo
    wb = np.zeros((128, 16, 5, 128), np.float32)
    qq = np.arange(128)
    for m in range(5):
        d = 128 * (4 - m) + (qq[None, :] - k[:, None])
        ok = (d >= 0) & (d < 512)
        for h in range(16):
            wb[:, h, m, :] = np.where(ok, -SLOPES[h] * d, -30000.0)
    tb["wbias"] = wb
    return tb


def core_tables(c):
    k = np.arange(128)
    tb = {}
    base = 1024 * c
    tq = base + np.arange(1024)
    cm = np.zeros((128, 4, 1024), np.float32)
    for ch in range(4):
        nn = 128 * ch + k
        end = 16 * nn + 31
        bad = (end[:, None] > tq[None, :]) | (nn[:, None] >= 511)
        cm[:, ch, :] = np.where(bad, -BIG, 0.0)
    tb["cmask"] = cm.astype(NPBF)
    kbc = np.zeros((128, 16, 2, 4), np.float32)
    kbh = np.zeros((128, 16, 2, 56), np.float32)
    for h in range(16):
        for qt in range(2):
            tref = base + 512 * qt
            for ch in range(4):
                kbc[:, h, qt, ch] = SLOPES[h] * (16 * (128 * ch + k) + 31 - tref)
            for jj in range(56):
                kbh[:, h, qt, jj] = SLOPES[h] * (128 * jj + k - tref)
    tb["kb_c"] = kbc
    tb["kb_h"] = kbh
    cur = (tq // 64)
    kk = np.arange(128)
    valid = (kk[None, :] <= cur[:, None]).astype(np.float32)
    forced = ((kk[None, :] == 0) | (kk[None, :] == cur[:, None]) | (kk[None, :] == cur[:, None] - 1)).astype(np.float32)
    addt = valid - 1.0 + 1e4 * forced
    tb["valid_t"] = valid.reshape(8, 128, 128).transpose(1, 0, 2).copy()
    tb["addt"] = addt.reshape(8, 128, 128).transpose(1, 0, 2).copy()
    hc = np.where((kk < 16 * c) & (kk < 112), 0.0, -BIG).astype(np.float32)
    tb["histcap"] = hc.reshape(128, 1)
    pc = np.zeros((128, 16), np.float32)
    for b in range(16):
        pc[16 * c + b, b] = 1.0
    tb["pc"] = pc.astype(NPBF)
    hb = np.zeros((128, 12), np.float32)
    if c == 0:
        hb[:, 0:4] = -30000.0
    tb["halo_bias"] = hb
    ic = np.zeros((4, 1024), np.float32)
    for gi, w in enumerate(POOL_WINDOWS):
        ic[gi] = 1.0 / np.minimum(tq + 1.0, float(w))
    tb["invcnt"] = ic
    return tb


STATIC_SPECS = [("ident_f", [128, 128], F32), ("ident_b", [128, 128], BF16), ("ones3", [3, 128], BF16),
                ("aq", [3, 16, 512], BF16), ("ovl1", [128, 4, 129], BF16), ("ehx", [128, 56, 128], BF16),
                ("eox", [19, 8, 128], BF16), ("cdiag", [128, 4, 512], BF16), ("kb_o", [128, 16, 2, 8], F32),
                ("wbias", [128, 16, 5, 128], F32)]
CORE_SPECS = [("cmask", [128, 4, 1024], BF16), ("kb_c", [128, 16, 2, 4], F32), ("kb_h", [128, 16, 2, 56], F32),
              ("valid_t", [128, 8, 128], F32), ("addt", [128, 8, 128], F32), ("histcap", [128, 1], F32),
              ("pc", [128, 16], BF16), ("halo_bias", [128, 12], F32), ("invcnt", [4, 1024], F32)]
WEIGHT_SPECS = [("x_all", [T, D]), ("x_ext", [NE, D]), ("pT", [256, NT]),
                ("g_mix_bc", [1, D]), ("g_ffn_bc", [1, D]), ("g_ple_bc", [1, D]), ("g_fin", [1, D]),
                ("w_in_a", [28, 128, 32, 256]), ("w_in_g", [1, 128, 32, 48]), ("w_in_b", [32, 128, 32, 256]), ("pool_w", [8, 128, 4, 256]), ("pool_scale", [128, 16]),
                ("posT", [128, 2, 32]), ("cmp_w_k", [4096, 128]), ("cmp_w_v", [4096, 128]),
                ("w_up_pool", [16, 128, 16, 256]), ("w_up_nsa", [16, 128, 16, 256]), ("w_out", [16, 128, 32, 256]),
                ("peer_w_q", [8, 128, 32, 256]), ("keysT", [128, 2, 128]), ("peer_uT", [64, 128, 32, 256]), ("peer_v", [16, 128, 128, 256]),
                ("ple_w_gate", [16, 128, 32, 256]), ("ple_w_proj", [16, 128, 2, 256])]


class Prog:
    def __init__(self, stop_after=None, debug=()):
        self.kb = KB()
        self.nc = self.kb.nc
        self.stop_after = stop_after
        self.debug = list(debug)
        self.dbg_specs = []
        nc = self.nc
        specs = {name: (shape, F32) for name, shape in WEIGHT_SPECS}
        specs.update({name: (shape, dt) for name, shape, dt in STATIC_SPECS + CORE_SPECS})
        prog = self

        class Lazy(dict):
            def __missing__(self, name):
                shape, dt = specs[name]
                ap = nc.dram_tensor(name, shape, dt, kind="ExternalInput").ap()
                self[name] = ap
                prog.used_inputs.append(name)
                return ap
        self.used_inputs = []
        self.dr = Lazy()
        self.dr["y"] = nc.dram_tensor("y", [NT, D], F32, kind="ExternalOutput").ap()

    def scratch(self, name, shape, dt):
        self.dr[name] = self.nc.dram_tensor(name, shape, dt, kind="Internal").ap()
        return self.dr[name]

    def dbg_out(self, name, shape, dt=F32):
        ap = self.nc.dram_tensor("dbg_" + name, shape, dt, kind="ExternalOutput").ap()
        self.dbg_specs.append("dbg_" + name)
        return ap

    def const(self, name, shape, dt, src=None, q="sp"):
        kb = self.kb
        t = kb.tile(name, shape, dt)
        s = self.dr[name] if src is None else src
        kb.dma(q, [(t.t[tuple(slice(None) for _ in shape)], s)], w=[t.b])
        return t

    def load_gfm(self, gname):
        kb = self.kb
        t = kb.tile("gbc", [128, D], F32)
        kb.dma("sp", [(t.t[:, :], self.dr[gname][0:1, :].to_broadcast([128, D]))], w=[t.b])
        return t

    def norm_setup(self, nxs=2, nscr=2):
        kb = self.kb
        self.xb = [kb.tile("xb", [128, D], F32) for _ in range(2)]
        self.xsb = [kb.tile("xsb", [128, D], BF16) for _ in range(nxs)]
        self.scr = [kb.tile("scr", [128, 1024], F32) for _ in range(nscr)]
        self.st = [kb.tile("st", [128, 12], F32) for _ in range(2)]

    def row_rstd(self, xt, s):
        kb, nc = self.kb, self.nc
        for i in range(4):
            scr = kb.rot("scr", self.scr)
            kb.op("act", lambda: nc.scalar.activation(out=scr.t[:, :], in_=xt.t[:, i * 1024:(i + 1) * 1024], func=AF.Square),
                  w=[scr.b], r=[xt.b])
            kb.op("dve", lambda: nc.vector.tensor_reduce(out=s.t[:, i:i + 1], in_=scr.t[:, :], axis=mybir.AxisListType.X, op=ALU.add),
                  w=[s.b], r=[scr.b])
        kb.op("dve", lambda: nc.vector.tensor_reduce(out=s.t[:, 6:7], in_=s.t[:, 0:4], axis=mybir.AxisListType.X, op=ALU.add), w=[s.b], r=[s.b])
        kb.op("dve", lambda: nc.vector.tensor_scalar(out=s.t[:, 7:8], in0=s.t[:, 6:7], scalar1=1.0 / D, scalar2=EPS,
                                                     op0=ALU.mult, op1=ALU.add), w=[s.b], r=[s.b])
        kb.op("act", lambda: nc.scalar.sqrt(out=s.t[:, 8:9], in_=s.t[:, 7:8]), w=[s.b], r=[s.b])
        kb.op("dve", lambda: nc.vector.reciprocal(out=s.t[:, 11:12], in_=s.t[:, 8:9]), w=[s.b], r=[s.b])

    def make_hT(self, xsrc, ntok, gbc, hT, tok_off=0):
        for tt in range(ntok // 128):
            self.make_hT_tile(xsrc, tt, gbc, hT, tok_off)

    def make_hT_tile(self, xsrc, tt, gbc, hT, tok_off=0):
        xs = self.make_hT_prep(xsrc, tt, gbc)
        self.make_hT_xpose(xs, tt, hT, tok_off)

    def make_hT_prep(self, xsrc, tt, gbc):
        kb, nc = self.kb, self.nc
        xt = kb.rot("xb", self.xb)
        xs = kb.rot("xsb", self.xsb)
        s = kb.rot("st", self.st)
        kb.dma("sp", [(xt.t[:, :], xsrc[tt * 128:(tt + 1) * 128, :])], w=[xt.b])
        self.row_rstd(xt, s)
        kb.op("dve", lambda: nc.vector.scalar_tensor_tensor(out=xs.t[:, :], in0=xt.t[:, :], scalar=s.t[:, 11:12], in1=gbc.t[:, :],
                                                            op0=ALU.mult, op1=ALU.mult), w=[xs.b], r=[xt.b, s.b, gbc.b])
        return xs

    def make_hT_xpose(self, xs, tt, hT, tok_off=0):
        kb, nc = self.kb, self.nc
        ident = self.ident_b
        for b in range(4):
            ps = kb.rot("psT", self.ps_misc)
            pv = ps.t[:, 0:512].bitcast(BF16)
            for j in range(8):
                kc = b * 8 + j
                kb.op("pe", lambda: nc.tensor.transpose(out=pv[:, j * 128:(j + 1) * 128],
                                                        in_=xs.t[:, kc * 128:(kc + 1) * 128], identity=ident.t[:, :]),
                      w=[ps.b], r=[xs.b, ident.b])
            o = hT.t[:, b * 8:(b + 1) * 8, tok_off + tt * 128: tok_off + (tt + 1) * 128]
            i = pv[:, 0:1024].rearrange("p (j t) -> p j t", j=8)
            self.evac("dve" if b % 2 == 0 else "act", o, i, w=[hT.b], r=[ps.b])

    def gemm_setup(self, nbuf=2):
        kb = self.kb
        self.wf = [kb.tile("wf", [128, 16, 256], F32) for _ in range(nbuf)]
        self.wb = [kb.tile("wb", [128, 16, 256], BF16) for _ in range(nbuf)]
        self.pf_dist = nbuf - 1

    def gemm(self, W, K, col0, ncols, ntok, act_fn, epi, banks=None, hook=None):
        kb, nc = self.kb, self.nc
        banks = banks if banks is not None else self.ps_acc
        nkc = K // 128
        kgs = [(k0, min(16, nkc - k0)) for k0 in range(0, nkc, 16)]
        ntt = (ntok + 511) // 512
        npan = (ncols + 255) // 256
        blocks = [(p, gi) for p in range(npan) for gi in range(len(kgs))]
        ready = {}

        def prefetch(bi):
            p, gi = blocks[bi]
            k0, nk = kgs[gi]
            pw = min(256, ncols - p * 256)
            wf = kb.rot("wf", self.wf)
            wb = kb.rot("wb", self.wb)
            kb.dma("sp", [(wf.t[:, 0:nk, 0:pw], W[col0 // 256 + p, :, k0:k0 + nk, 0:pw])], w=[wf.b])
            ce = kb.rot("cast", ["pool", "dve", "act"])
            o, i = wb.t[:, 0:nk, 0:pw], wf.t[:, 0:nk, 0:pw]
            if ce == "pool":
                kb.op("pool", lambda: nc.gpsimd.tensor_copy(out=o, in_=i), w=[wb.b], r=[wf.b])
            elif ce == "dve":
                kb.op("dve", lambda: nc.vector.tensor_copy(out=o, in_=i), w=[wb.b], r=[wf.b])
            else:
                kb.op("act", lambda: nc.scalar.copy(out=o, in_=i), w=[wb.b], r=[wf.b])
            ready[bi] = (wb, act_fn(gi))
        dist = self.pf_dist
        for b0 in range(min(dist, len(blocks))):
            prefetch(b0)
        accs = {}
        for bi, (p, gi) in enumerate(blocks):
            k0, nk = kgs[gi]
            pw = min(256, ncols - p * 256)
            nch = (pw + 127) // 128
            if gi == 0:
                if hook is not None:
                    hook(p)
                accs = {}
                for c in range(nch):
                    for tt in range(ntt):
                        accs[(c, tt)] = kb.rot("acc", banks)
            if bi + dist < len(blocks):
                prefetch(bi + dist)
            wb, (act_ap, act_b) = ready.pop(bi)
            for c in range(nch):
                cw = min(128, pw - c * 128)
                for tt in range(ntt):
                    n = min(512, ntok - tt * 512)
                    ps = accs[(c, tt)]
                    for kl in range(nk):
                        st_ = (gi == 0 and kl == 0)
                        sp_ = (gi == len(kgs) - 1 and kl == nk - 1)
                        kb.op("pe", lambda: nc.tensor.matmul(ps.t[0:cw, 0:n], lhsT=wb.t[:, kl, c * 128:c * 128 + cw],
                                                             rhs=act_ap(kl, tt * 512, n), start=st_, stop=sp_),
                              w=[ps.b], r=[wb.b, act_b])
            if gi == len(kgs) - 1:
                for c in range(nch):
                    cw = min(128, pw - c * 128)
                    for tt in range(ntt):
                        n = min(512, ntok - tt * 512)
                        epi(p * 2 + c, tt, accs[(c, tt)], cw, n)

    def resident_act(self, tile, kc0=0):
        def f(gi):
            def ap(kl, t0, n):
                return tile.t[:, kc0 + gi * 16 + kl, t0:t0 + n]
            return ap, tile.b
        return f

    def common_consts(self):
        kb = self.kb
        self.ps_misc = kb.ps[6:8]
        self.ps_acc = kb.ps[0:6]
        self.ident_f = self.const("ident_f", [128, 128], F32)
        self.ident_b = self.const("ident_b", [128, 128], BF16)

    def evac(self, eng, o, i, w, r, scale=None):
        kb, nc = self.kb, self.nc
        if eng == "dve":
            if scale is None:
                kb.op("dve", lambda: nc.vector.tensor_copy(out=o, in_=i), w=w, r=r)
            else:
                kb.op("dve", lambda: nc.vector.tensor_scalar(out=o, in0=i, scalar1=scale, scalar2=None, op0=ALU.mult), w=w, r=r)
        else:
            if scale is None:
                kb.op("act", lambda: nc.scalar.copy(out=o, in_=i), w=w, r=r)
            else:
                kb.op("dve", lambda: nc.vector.tensor_scalar(out=o, in0=i, scalar1=scale, scalar2=None, op0=ALU.mult), w=w, r=r)

    def fm_to_tm_bf16(self, src, n, dst_fn, dst_b):
        kb, nc = self.kb, self.nc
        ps = kb.rot("psT", self.ps_misc)
        nsub = n // 128
        pv = ps.t[:, 0:256].bitcast(BF16)
        for j in range(nsub):
            kb.op("pe", lambda: nc.tensor.transpose(out=pv[:, j * 128:(j + 1) * 128], in_=src.t[:, j * 128:(j + 1) * 128],
                                                    identity=self.ident_b.t[:, :]), w=[ps.b], r=[src.b, self.ident_b.b])
        for j in range(nsub):
            self.evac("dve" if j % 2 == 0 else "act", dst_fn(j), pv[:, j * 128:(j + 1) * 128], w=[dst_b], r=[ps.b])

    def phase_kv_all(self):
        kb, nc, dr = self.kb, self.nc, self.dr
        kb.phase()
        self.common_consts()
        self.norm_setup(1, 1)
        self.gemm_setup(3)
        gfm = self.load_gfm("g_mix_bc")
        posT = self.const("posT", [128, 2, 32], F32)
        cmpAB = self.scratch("cmpAB", [2, 4, 2, 128, T], BF16)
        kslcT = self.scratch("kslcT_all", [4, 128, T], BF16)
        vslc = self.scratch("vslc_all", [T, 4, 128], BF16)
        hTs = [kb.tile("hT", [128, 32, 512], BF16) for _ in range(2)]
        stAB = [kb.tile("stAB", [128, 2, 512], BF16) for _ in range(2)]
        stK = [kb.tile("stK", [128, 512], BF16) for _ in range(2)]
        stV = [kb.tile("stV", [128, 4, 128], BF16) for _ in range(2)]
        import os
        NG = int(os.environ.get("KV_GROUPS", T // 512))
        self.make_hT(dr["x_all"][0:512, :], 512, gfm, hTs[0])
        pend_xs = [None]
        for G in range(NG):
            hT = hTs[G % 2]

            def epi(ch, tt, ps, cw, n, G=G):
                kvi, g = ch // 4, ch % 4
                tok0 = G * 512
                if kvi < 2:
                    s = kb.rot("stAB", stAB)
                    pv = ps.t[:, 0:512].rearrange("p (a l) -> p a l", l=16)
                    for ab in range(2):
                        o = s.t[:, ab, :].rearrange("p (a l) -> p a l", l=16)
                        pb = posT.t[:, kvi, ab * 16:(ab + 1) * 16].unsqueeze(1).to_broadcast([128, 32, 16])
                        kb.op("dve", lambda: nc.vector.tensor_tensor(out=o, in0=pv, in1=pb, op=ALU.add), w=[s.b], r=[ps.b, posT.b])
                    kb.dma("pool", [(cmpAB[kvi, g, :, :, tok0:tok0 + 512].rearrange("a p t -> p a t"), s.t[:, :, :])], r=[s.b])
                elif kvi == 2:
                    s = kb.rot("stK", stK)
                    self.evac("act", s.t[:, :], ps.t[:, 0:512], w=[s.b], r=[ps.b])
                    kb.dma("pool", [(kslcT[g, :, tok0:tok0 + 512], s.t[:, :])], r=[s.b])
                else:
                    s = kb.rot("stK", stK)
                    self.evac("act", s.t[:, :], ps.t[:, 0:512], w=[s.b], r=[ps.b])
                    sv = kb.rot("stV", stV)
                    self.fm_to_tm_bf16(s, 512, lambda j: sv.t[:, j, :], sv.b)
                    kb.dma("pool", [(vslc[tok0:tok0 + 512, g, :].rearrange("(j p) d -> p j d", p=128), sv.t[:, :, :])], r=[sv.b])

            def hook(p, G=G):
                if G + 1 < NG:
                    if p % 2 == 0:
                        pend_xs[0] = self.make_hT_prep(dr["x_all"][(G + 1) * 512:(G + 2) * 512, :], p // 2, gfm)
                    else:
                        self.make_hT_xpose(pend_xs[0], p // 2, hTs[(G + 1) % 2])
            self.gemm(dr["w_in_a"], D, 4096, 2048, 512, self.resident_act(hT), epi, hook=hook)

    def phase_own(self):
        kb, nc, dr = self.kb, self.nc, self.dr
        kb.phase()
        self.common_consts()
        self.gemm_setup()
        gfm = self.load_gfm("g_mix_bc")
        qT_d = self.scratch("qT_d", [16, 128, NT], BF16)
        kslcT_o = self.scratch("kslcT_o", [4, 128, NT], BF16)
        vslc_o = self.scratch("vslc_o", [NT, 4, 128], BF16)
        kwinT = self.scratch("kwinT", [4, 128, NE], BF16)
        vwin = self.scratch("vwin", [NE, 4, 128], BF16)
        gates_d = self.scratch("gates_d", [NT, 48], F32)
        gpT = self.scratch("gpT", [D, NT], BF16)
        gnT = self.scratch("gnT", [D, NT], BF16)
        pooledT_d = self.scratch("pooledT_d", [16, 128, NT], BF16)
        uh = kb.tile("uhalo", [128, 16, 16], F32)
        stK = [kb.tile("stK", [128, 512], BF16) for _ in range(2)]
        stV = [kb.tile("stV", [128, 4, 128], BF16) for _ in range(2)]
        stF = [kb.tile("stF", [128, 512], F32) for _ in range(2)]
        stG = [kb.tile("stG", [128, 4, 48], F32) for _ in range(2)]
        hT = kb.tile("hT", [128, 32, NT], BF16)

        def kv_epi(kvi, g, tok0, ps, n):
            s = kb.rot("stK", stK)
            self.evac("act", s.t[:, 0:n], ps.t[:, 0:n], w=[s.b], r=[ps.b])
            if kvi == 2:
                kb.dma("pool", [(kslcT_o[g, :, tok0 - NH:tok0 - NH + n], s.t[:, 0:n])], r=[s.b])
            elif kvi == 4:
                kb.dma("pool", [(kwinT[g, :, tok0:tok0 + n], s.t[:, 0:n])], r=[s.b])
            else:
                sv = kb.rot("stV", stV)
                self.fm_to_tm_bf16(s, n, lambda j: sv.t[:, j, :], sv.b)
                if kvi == 3:
                    dst = vslc_o[tok0 - NH:tok0 - NH + n, g, :]
                else:
                    dst = vwin[tok0:tok0 + n, g, :]
                kb.dma("pool", [(dst.rearrange("(j p) d -> p j d", p=128), sv.t[:, 0:n // 128, :])], r=[sv.b])

        kb.push()
        self.norm_setup()
        self.make_hT(dr["x_ext"][0:NH, :], NH, gfm, hT)
        kb.pop()

        def epi_h_pool(ch, tt, ps, cw, n):
            self.evac("dve", uh.t[:, ch, :], ps.t[:, 496:512], w=[uh.b], r=[ps.b])
        self.gemm(dr["w_in_a"], D, 0, 2048, NH, self.resident_act(hT), epi_h_pool)

        def epi_h_win(ch, tt, ps, cw, n):
            kv_epi(4 + ch // 4, ch % 4, 0, ps, n)
        self.gemm(dr["w_in_a"], D, 4096 + 2048, 1024, NH, self.resident_act(hT), epi_h_win)

        kb.push()
        self.norm_setup()
        self.make_hT(dr["x_ext"][NH:NE, :], NT, gfm, hT)
        kb.pop()

        U = [kb.tile("U", [128, 16 + NT], F32) for _ in range(3)]
        icb = [kb.tile("icb", [128, NT], F32) for _ in range(2)]
        stP = [kb.tile("stP", [128, NT], BF16) for _ in range(2)]
        pend = {}

        def epi_pool(ch, tt, ps, cw, n):
            gi = ch // 4
            if tt == 0:
                u0 = kb.rot("U", U)
                pend[ch] = u0
                kb.op("dve", lambda: nc.vector.tensor_copy(out=u0.t[:, 0:16], in_=uh.t[:, ch, :]), w=[u0.b], r=[uh.b])
            u0 = pend[ch]
            self.evac("act", u0.t[:, 16 + tt * 512:16 + tt * 512 + n], ps.t[:, 0:n], w=[u0.b], r=[ps.b])
            if tt == 1:
                cur = u0
                L = 16 + NT
                sh = 1
                others = [u for u in U if u is not u0]
                for step in range(gi + 1):
                    nxt = others[step % 2]
                    kb.op("dve", lambda: nc.vector.tensor_tensor(out=nxt.t[:, sh:L], in0=cur.t[:, sh:L], in1=cur.t[:, 0:L - sh],
                                                                 op=ALU.add), w=[nxt.b], r=[cur.b])
                    if step >= 1 and cur is not u0:
                        pass
                    cur = nxt
                    sh *= 2
                ic = kb.rot("icb", icb)
                kb.dma("sp", [(ic.t[:, :], dr["invcnt"][gi:gi + 1, :].to_broadcast([128, NT]))], w=[ic.b])
                kb.op("dve", lambda: nc.vector.tensor_tensor(out=cur.t[:, 16:L], in0=cur.t[:, 16:L], in1=ic.t[:, :], op=ALU.mult),
                      w=[cur.b], r=[ic.b])
                sp = kb.rot("stP", stP)
                kb.op("dve", lambda: nc.vector.tensor_tensor(out=sp.t[:, :], in0=cur.t[:, 16:L], in1=u0.t[:, 16:L], op=ALU.subtract),
                      w=[sp.b], r=[cur.b, u0.b])
                kb.dma("pool", [(pooledT_d[ch, :, :], sp.t[:, :])], r=[sp.b])
        self.gemm(dr["w_in_a"], D, 0, 2048, NT, self.resident_act(hT), epi_pool)

        def epi_q(ch, tt, ps, cw, n):
            s = kb.rot("stK", stK)
            kb.op("act", lambda: nc.scalar.activation(out=s.t[:, 0:n], in_=ps.t[:, 0:n], func=AF.Copy, scale=128.0 ** -0.5),
                  w=[s.b], r=[ps.b])
            kb.dma("pool", [(qT_d[ch, :, tt * 512:tt * 512 + n], s.t[:, 0:n])], r=[s.b])
        self.gemm(dr["w_in_a"], D, 2048, 2048, NT, self.resident_act(hT), epi_q)

        def epi_kv(ch, tt, ps, cw, n):
            kv_epi(2 + ch // 4, ch % 4, NH + tt * 512, ps, n)
        self.gemm(dr["w_in_a"], D, 4096 + 1024, 2048, NT, self.resident_act(hT), epi_kv)

        def epi_gates(ch, tt, ps, cw, n):
            s = kb.rot("stF", stF)
            self.evac("dve", s.t[0:48, 0:n], ps.t[0:48, 0:n], w=[s.b], r=[ps.b])
            p2 = kb.rot("psT", self.ps_misc)
            for j in range(4):
                kb.op("pe", lambda: nc.tensor.transpose(out=p2.t[:, j * 48:(j + 1) * 48], in_=s.t[0:48, j * 128:(j + 1) * 128],
                                                        identity=self.ident_f.t[0:48, 0:48]), w=[p2.b], r=[s.b, self.ident_f.b])
            sg = kb.rot("stG", stG)
            kb.op("act", lambda: nc.scalar.activation(out=sg.t[:, :, :], in_=p2.t[:, 0:192].rearrange("p (j c) -> p j c", j=4),
                                                      func=AF.Sigmoid), w=[sg.b], r=[p2.b])
            kb.dma("pool", [(gates_d[tt * 512:(tt + 1) * 512, :].rearrange("(j p) c -> p j c", p=128), sg.t[:, :, :])], r=[sg.b])
        self.gemm(dr["w_in_g"], D, 0, 48, NT, self.resident_act(hT), epi_gates)

        def mk_epi_sig(dst):
            def epi(ch, tt, ps, cw, n):
                s = kb.rot("stK", stK)
                kb.op("act", lambda: nc.scalar.activation(out=s.t[:, 0:n], in_=ps.t[:, 0:n], func=AF.Sigmoid), w=[s.b], r=[ps.b])
                kb.dma("pool", [(dst[ch * 128:(ch + 1) * 128, tt * 512:tt * 512 + n], s.t[:, 0:n])], r=[s.b])
            return epi
        self.gemm(dr["w_in_b"], D, 0, 4096, NT, self.resident_act(hT), mk_epi_sig(gpT))
        self.gemm(dr["w_in_b"], D, 4096, 4096, NT, self.resident_act(hT), mk_epi_sig(gnT))

    def phase_poolg(self):
        kb, nc, dr = self.kb, self.nc, self.dr
        kb.phase()
        self.common_consts()
        self.gemm_setup()
        poT_d = self.scratch("poT_d", [16, 128, NT], BF16)
        pooled = kb.tile("pooled", [128, 16, NT], BF16)
        kb.dma("sp", [(pooled.t[:, :, :], dr["pooledT_d"].rearrange("c p t -> p c t"))], w=[pooled.b])
        psc = self.const("pool_scale", [128, 16], F32)
        stK = [kb.tile("stK", [128, 512], BF16) for _ in range(2)]
        for g in range(4):
            def epi(ch, tt, ps, cw, n, g=g):
                s = kb.rot("stK", stK)
                c16 = g * 4 + ch
                self.evac("act", s.t[:, 0:n], ps.t[:, 0:n], w=[s.b], r=[ps.b], scale=psc.t[:, c16:c16 + 1])
                kb.dma("pool", [(poT_d[c16, :, tt * 512:tt * 512 + n], s.t[:, 0:n])], r=[s.b])
            self.gemm(dr["pool_w"], 512, g * 512, 512, NT, self.resident_act(pooled, kc0=g * 4), epi)

    def phase_cmp(self):
        kb, nc, dr = self.kb, self.nc, self.dr
        kb.phase()
        self.common_consts()
        kcT_d = self.scratch("kcT_d", [4, 128, 512], BF16)
        vc_d = self.scratch("vc_d", [512, 4, 128], BF16)
        wcf = kb.tile("wcf", [128, 32, 128], F32)
        wcb = [kb.tile("wcb", [128, 32, 128], BF16) for _ in range(2)]
        AB = [[kb.tile("cA", [128, T], BF16), kb.tile("cB", [128, T], BF16)] for _ in range(2)]
        stk = [kb.tile("stk", [128, 512], BF16) for _ in range(2)]
        stv = [kb.tile("stv", [128, 128], BF16) for _ in range(2)]
        for s in stk + stv:
            kb.op("dve", lambda: nc.vector.memset(s.t[:, :], 0.0), w=[s.b])
        for kv in range(2):
            wsrc = dr["cmp_w_k" if kv == 0 else "cmp_w_v"]
            kb.dma("sp", [(wcf.t[:, :, :], wsrc.rearrange("(l d) c -> d l c", d=128))], w=[wcf.b])
            wb = wcb[kv]
            kb.op("dve", lambda: nc.vector.tensor_copy(out=wb.t[:, :, :], in_=wcf.t[:, :, :]), w=[wb.b], r=[wcf.b])
            for g in range(4):
                A, B = AB[g % 2]
                kb.dma("sp", [(A.t[:, :], dr["cmpAB"][kv, g, 0, :, :])], w=[A.b])
                kb.dma("sp", [(B.t[:, :], dr["cmpAB"][kv, g, 1, :, :])], w=[B.b])
                Av = A.t[:, :].rearrange("p (n s) -> p n s", s=16)
                Bv = B.t[:, :].rearrange("p (n s) -> p n s", s=16)

                def view(l, n0, n1):
                    if l < 16:
                        return Av[:, n0:n1, l], A.b
                    return Bv[:, n0 + 1:n1 + 1, l - 16], B.b
                if kv == 0:
                    ps = kb.rot("acc", self.ps_acc)
                    for l in range(32):
                        v, vb = view(l, 0, 511)
                        kb.op("pe", lambda: nc.tensor.matmul(ps.t[:, 0:511], lhsT=wb.t[:, l, :], rhs=v, start=(l == 0), stop=(l == 31)),
                              w=[ps.b], r=[wb.b, vb])
                    s = kb.rot("stk", stk)
                    self.evac("act", s.t[:, 0:511], ps.t[:, 0:511], w=[s.b], r=[ps.b])
                    kb.dma("pool", [(kcT_d[g, :, :], s.t[:, :])], r=[s.b])
                else:
                    for ch in range(4):
                        M = 128 if ch < 3 else 127
                        ps = kb.rot("acc", self.ps_acc)
                        for l in range(32):
                            v, vb = view(l, ch * 128, ch * 128 + M)
                            kb.op("pe", lambda: nc.tensor.matmul(ps.t[0:M, 0:128], lhsT=v, rhs=wb.t[:, l, :], start=(l == 0), stop=(l == 31)),
                                  w=[ps.b], r=[wb.b, vb])
                        s = kb.rot("stv", stv)
                        self.evac("dve", s.t[0:M, :], ps.t[0:M, 0:128], w=[s.b], r=[ps.b])
                        kb.dma("pool", [(vc_d[ch * 128:(ch + 1) * 128, g, :], s.t[:, :])], r=[s.b])

    def phase_att(self):
        kb, nc, dr = self.kb, self.nc, self.dr
        kb.phase()
        self.common_consts()
        psS = kb.ps[0:2]
        psS3 = kb.ps[0:3]
        psO = kb.ps[3:7]
        psW = kb.ps[0:6]
        psPW = kb.ps[6:8]
        self.ps_misc = kb.ps[7:8]
        nsaT_d = self.scratch("nsaT_d", [16, 128, NT], BF16)
        ones3 = self.const("ones3", [3, 128], BF16)
        aq = self.const("aq", [3, 16, 512], BF16)
        ehx = self.const("ehx", [128, 56, 128], BF16)
        eox = self.const("eox", [19, 8, 128], BF16)
        selHx = kb.tile("selHx", [128, 4, 512], BF16)
        selOx = kb.tile("selOx", [19, 4, 512], BF16)
        cdiag = self.const("cdiag", [128, 4, 512], BF16)
        kb_o = self.const("kb_o", [128, 16, 2, 8], F32)
        kb_c = self.const("kb_c", [128, 16, 2, 4], F32)
        kb_h = self.const("kb_h", [128, 16, 2, 56], F32)
        cmask = self.const("cmask", [128, 4, 1024], BF16)
        valid_t = self.const("valid_t", [128, 8, 128], F32)
        addt = self.const("addt", [128, 8, 128], F32)
        histcap = self.const("histcap", [128, 1], F32)
        pc = self.const("pc", [128, 16], BF16)
        halo_bias = self.const("halo_bias", [128, 12], F32)
        gates = self.const("gates", [128, 8, 48], F32, src=dr["gates_d"].rearrange("(j p) c -> p j c", p=128))
        ident_b, ident_f = self.ident_b, self.ident_f

        qg = kb.tile("qg", [128, 4, NT], BF16)
        kh = kb.tile("kh", [128, 7168], BF16)
        Vh = kb.tile("Vh", [128, 56, 129], BF16)
        ko = kb.tile("ko", [128, NT], BF16)
        Vo = kb.tile("Vo", [128, 8, 129], BF16)
        kw = kb.tile("kw", [128, NE], BF16)
        Vw = kb.tile("Vw", [128, 12, 129], BF16)
        kc = kb.tile("kc", [128, 512], BF16)
        Rc = kb.tile("Rc", [128, 4, 257], BF16)
        wbg = kb.tile("wbg", [128, 4, 5, 128], F32)
        for V in (Vh, Vo, Vw):
            kb.op("dve", lambda: nc.vector.memset(V.t[:, :, 128:129], 1.0), w=[V.b])
        PTc = [kb.tile("PTc", [128, 4, 512], BF16) for _ in range(2)]
        PTs = [kb.tile("PTs", [128, 512], BF16) for _ in range(3)]
        PTw = [kb.tile("PTw", [128, 5, 128], BF16) for _ in range(3)]
        Sb = [kb.tile("Sb", [128, 5, 128], F32) for _ in range(3)]
        zts = [kb.tile("zt", [128, 4], F32) for _ in range(4)]
        oacc = kb.tile("oacc", [128, 4, 4, 128], F32)
        imp = kb.tile("imp", [128, 4, 128], F32)
        score = [kb.tile("score", [128, 128], F32) for _ in range(2)]
        work = [kb.tile("work", [128, 128], F32) for _ in range(2)]
        m8 = [kb.tile("m8", [128, 16], F32) for _ in range(2)]
        selb = [kb.tile("selb", [128, 128], BF16) for _ in range(2)]
        selT = kb.tile("selT", [128, 512], BF16)
        selTh = kb.tile("selTh", [128, 512], BF16)
        selTo = kb.tile("selTo", [16, 512], BF16)
        stN = [kb.tile("stN", [128, 4, 512], BF16) for _ in range(2)]

        def fac(p2ap, p2b, zcol, s8, gcol, guard):
            zt = kb.rot("zt", zts)
            if guard:
                kb.op("dve", lambda: nc.vector.tensor_scalar(out=zt.t[:, 0:1], in0=p2ap[:, zcol:zcol + 1], scalar1=1e-30, scalar2=None,
                                                             op0=ALU.max), w=[zt.b], r=[p2b])
                kb.op("dve", lambda: nc.vector.reciprocal(out=zt.t[:, 1:2], in_=zt.t[:, 0:1]), w=[zt.b], r=[zt.b])
            else:
                kb.op("dve", lambda: nc.vector.reciprocal(out=zt.t[:, 1:2], in_=p2ap[:, zcol:zcol + 1]), w=[zt.b], r=[p2b])
            kb.op("dve", lambda: nc.vector.tensor_tensor(out=zt.t[:, 2:3], in0=zt.t[:, 1:2], in1=gates.t[:, s8, gcol:gcol + 1], op=ALU.mult),
                  w=[zt.b], r=[zt.b, gates.b])
            return zt

        for g in range(4):
            kb.dma("sp", [(qg.t[:, :, :], dr["qT_d"][4 * g:4 * g + 4, :, :].rearrange("h p t -> p h t"))], w=[qg.b])
            kb.dma("sp", [(kh.t[:, :], dr["kslcT_all"][g, :, 0:7168])], w=[kh.b])
            kb.dma("sp", [(Vh.t[:, :, 0:128], dr["vslc_all"][0:7168, g, :].rearrange("(j p) d -> p j d", p=128))], w=[Vh.b])
            kb.dma("sp", [(ko.t[:, :], dr["kslcT_o"][g, :, :])], w=[ko.b])
            kb.dma("sp", [(Vo.t[:, :, 0:128], dr["vslc_o"][:, g, :].rearrange("(j p) d -> p j d", p=128))], w=[Vo.b])
            kb.dma("sp", [(kw.t[:, :], dr["kwinT"][g, :, :])], w=[kw.b])
            kb.dma("sp", [(Vw.t[:, :, 0:128], dr["vwin"][:, g, :].rearrange("(j p) d -> p j d", p=128))], w=[Vw.b])
            kb.dma("sp", [(kc.t[:, :], dr["kcT_d"][g, :, :])], w=[kc.b])
            kb.dma("sp", [(Rc.t[:, :, 0:128], dr["vc_d"][:, g, :].rearrange("(j p) d -> p j d", p=128)),
                          (Rc.t[:, :, 128:257], dr["ovl1"])], w=[Rc.b])
            kb.dma("sp", [(wbg.t[:, :, :, :], dr["wbias"][:, 4 * g:4 * g + 4, :, :])], w=[wbg.b])
            for qt in range(2):
                q0 = qt * 512
                for hl in range(4):
                    hh = 4 * g + hl
                    pt = kb.rot("PTc", PTc)
                    for ch in range(4):
                        ps = kb.rot("psS", psS)
                        kb.op("pe", lambda: nc.tensor.matmul(ps.t[:, 0:512], lhsT=kc.t[:, ch * 128:(ch + 1) * 128], rhs=qg.t[:, hl, q0:q0 + 512],
                                                             start=True, stop=False), w=[ps.b], r=[kc.b, qg.b])
                        kb.op("pe", lambda: nc.tensor.matmul(ps.t[:, 0:512], lhsT=ones3.t[0:3, :], rhs=aq.t[0:3, hh, :],
                                                             start=False, stop=False), w=[ps.b], r=[ones3.b, aq.b])
                        kb.op("pe", lambda: nc.tensor.matmul(ps.t[:, 0:512], lhsT=ident_b.t[:, :], rhs=cmask.t[:, ch, q0:q0 + 512],
                                                             start=False, stop=True), w=[ps.b], r=[ident_b.b, cmask.b])
                        kb.op("act", lambda: nc.scalar.activation(out=pt.t[:, ch, :], in_=ps.t[:, 0:512], func=AF.Exp,
                                                                  bias=kb_c.t[:, hh, qt, ch:ch + 1]), w=[pt.b], r=[ps.b, kb_c.b])
                    for sub in range(4):
                        s8 = qt * 4 + sub
                        p2 = kb.rot("psT", self.ps_misc)
                        for ch in range(4):
                            kb.op("pe", lambda: nc.tensor.matmul(p2.t[:, 0:257], lhsT=pt.t[:, ch, sub * 128:(sub + 1) * 128], rhs=Rc.t[:, ch, :],
                                                                 start=(ch == 0), stop=(ch == 3)), w=[p2.b], r=[pt.b, Rc.b])
                        zt = fac(p2.t, p2.b, 256, s8, hh * 3 + 0, True)
                        kb.op("dve", lambda: nc.vector.tensor_scalar(out=oacc.t[:, sub, hl, :], in0=p2.t[:, 0:128], scalar1=zt.t[:, 2:3],
                                                                     scalar2=None, op0=ALU.mult), w=[oacc.b], r=[p2.b, zt.b])
                        if hl == 0:
                            kb.op("dve", lambda: nc.vector.tensor_scalar(out=imp.t[:, sub, :], in0=p2.t[:, 128:256], scalar1=zt.t[:, 1:2],
                                                                         scalar2=None, op0=ALU.mult), w=[imp.b], r=[p2.b, zt.b])
                        else:
                            kb.op("dve", lambda: nc.vector.scalar_tensor_tensor(out=imp.t[:, sub, :], in0=p2.t[:, 128:256], scalar=zt.t[:, 1:2],
                                                                                in1=imp.t[:, sub, :], op0=ALU.mult, op1=ALU.add),
                                  w=[imp.b], r=[p2.b, zt.b])
                for sub in range(4):
                    s8 = qt * 4 + sub
                    sc = kb.rot("score", score)
                    wk = kb.rot("work", work)
                    mm = kb.rot("m8", m8)
                    sb = kb.rot("selb", selb)
                    kb.op("dve", lambda: nc.vector.tensor_tensor(out=sc.t[:, :], in0=imp.t[:, sub, :], in1=valid_t.t[:, s8, :], op=ALU.mult),
                          w=[sc.b], r=[imp.b, valid_t.b])
                    kb.op("dve", lambda: nc.vector.tensor_tensor(out=sc.t[:, :], in0=sc.t[:, :], in1=addt.t[:, s8, :], op=ALU.add),
                          w=[sc.b], r=[addt.b])
                    kb.op("dve", lambda: nc.vector.max(out=mm.t[:, 0:8], in_=sc.t[:, :]), w=[mm.b], r=[sc.b])
                    kb.op("dve", lambda: nc.vector.match_replace(out=wk.t[:, :], in_to_replace=mm.t[:, 0:8], in_values=sc.t[:, :], imm_value=-1e30),
                          w=[wk.b], r=[mm.b, sc.b])
                    kb.op("dve", lambda: nc.vector.max(out=mm.t[:, 8:16], in_=wk.t[:, :]), w=[mm.b], r=[wk.b])
                    kb.op("dve", lambda: nc.vector.scalar_tensor_tensor(out=wk.t[:, :], in0=sc.t[:, :], scalar=mm.t[:, 15:16], in1=valid_t.t[:, s8, :],
                                                                        op0=ALU.is_ge, op1=ALU.mult), w=[wk.b], r=[sc.b, mm.b, valid_t.b])
                    kb.op("dve", lambda: nc.vector.tensor_scalar(out=sb.t[:, :], in0=wk.t[:, :], scalar1=BIG, scalar2=-BIG, op0=ALU.mult, op1=ALU.add),
                          w=[sb.b], r=[wk.b])
                    p2 = kb.rot("psT", self.ps_misc)
                    pv = p2.t[:, 0:64].bitcast(BF16)
                    kb.op("pe", lambda: nc.tensor.transpose(out=pv[:, 0:128], in_=sb.t[:, :], identity=ident_b.t[:, :]),
                          w=[p2.b], r=[sb.b, ident_b.b])
                    self.evac("act", selT.t[:, sub * 128:(sub + 1) * 128], pv[:, 0:128], w=[selT.b], r=[p2.b])
                kb.op("dve", lambda: nc.vector.tensor_scalar(out=selTh.t[:, :], in0=selT.t[:, :], scalar1=histcap.t[:, 0:1], scalar2=None, op0=ALU.min),
                      w=[selTh.b], r=[selT.b, histcap.b])
                p2 = kb.rot("psT", self.ps_misc)
                kb.op("pe", lambda: nc.tensor.matmul(p2.t[0:16, 0:512], lhsT=pc.t[:, 0:16], rhs=selT.t[:, :], start=True, stop=True),
                      w=[p2.b], r=[pc.b, selT.b])
                self.evac("act", selTo.t[0:16, :], p2.t[0:16, 0:512], w=[selTo.b], r=[p2.b])
                for hl in range(4):
                    hh = 4 * g + hl
                    kb.op("pool" if hl % 2 else "dve",
                          (lambda: nc.gpsimd.tensor_copy(out=selHx.t[:, hl, :], in_=selTh.t[:, :])) if hl % 2 else
                          (lambda: nc.vector.tensor_copy(out=selHx.t[:, hl, :], in_=selTh.t[:, :])), w=[selHx.b], r=[selTh.b])
                    kb.op("act", lambda: nc.scalar.copy(out=selOx.t[0:16, hl, :], in_=selTo.t[0:16, :]), w=[selOx.b], r=[selTo.b])
                kb.dma("sp", [(selHx.t[112:115, :, :], dr["aq"][:, 4 * g:4 * g + 4, :]),
                              (selOx.t[16:19, :, :], dr["aq"][:, 4 * g:4 * g + 4, :])], w=[selHx.b, selOx.b])
                for hl in range(4):
                    hh = 4 * g + hl
                    chunks = [("h", j) for j in range(56)] + [("o", j) for j in range(4 * qt + 4)]
                    last = len(chunks) - 1
                    pss = {}

                    def scores(idx):
                        kind, j = chunks[idx]
                        ps = kb.rot("psS3", psS3)
                        pss[idx] = ps
                        ksrc = kh if kind == "h" else ko
                        kb.op("pe", lambda: nc.tensor.matmul(ps.t[:, 0:512], lhsT=ksrc.t[:, j * 128:(j + 1) * 128], rhs=qg.t[:, hl, q0:q0 + 512],
                                                             start=True, stop=False), w=[ps.b], r=[ksrc.b, qg.b])
                        if kind == "h":
                            kb.op("pe", lambda: nc.tensor.matmul(ps.t[:, 0:512], lhsT=ehx.t[0:115, j, :], rhs=selHx.t[0:115, hl, :], start=False, stop=True),
                                  w=[ps.b], r=[ehx.b, selHx.b])
                        else:
                            diag = j >= 4 * qt
                            kb.op("pe", lambda: nc.tensor.matmul(ps.t[:, 0:512], lhsT=eox.t[0:19, j, :], rhs=selOx.t[0:19, hl, :], start=False, stop=not diag),
                                  w=[ps.b], r=[eox.b, selOx.b])
                            if diag:
                                kb.op("pe", lambda: nc.tensor.matmul(ps.t[:, 0:512], lhsT=ident_b.t[:, :], rhs=cdiag.t[:, j - 4 * qt, :],
                                                                     start=False, stop=True), w=[ps.b], r=[ident_b.b, cdiag.b])
                    scores(0)
                    scores(1)
                    for idx, (kind, j) in enumerate(chunks):
                        if idx + 2 <= last:
                            scores(idx + 2)
                        ps = pss.pop(idx)
                        if kind == "h":
                            bias, bb, Vt = kb_h.t[:, hh, qt, j:j + 1], kb_h.b, Vh
                        else:
                            bias, bb, Vt = kb_o.t[:, hh, qt, j:j + 1], kb_o.b, Vo
                        pt = kb.rot("PTs", PTs)
                        kb.op("act", lambda: nc.scalar.activation(out=pt.t[:, :], in_=ps.t[:, 0:512], func=AF.Exp, bias=bias),
                              w=[pt.b], r=[ps.b, bb])
                        for sub in range(4):
                            po = psO[sub]
                            kb.op("pe", lambda: nc.tensor.matmul(po.t[:, 0:129], lhsT=pt.t[:, sub * 128:(sub + 1) * 128], rhs=Vt.t[:, j, :],
                                                                 start=(idx == 0), stop=(idx == last)), w=[po.b], r=[pt.b, Vt.b])
                    for sub in range(4):
                        s8 = qt * 4 + sub
                        po = psO[sub]
                        zt = fac(po.t, po.b, 128, s8, hh * 3 + 1, False)
                        kb.op("dve", lambda: nc.vector.scalar_tensor_tensor(out=oacc.t[:, sub, hl, :], in0=po.t[:, 0:128], scalar=zt.t[:, 2:3],
                                                                            in1=oacc.t[:, sub, hl, :], op0=ALU.mult, op1=ALU.add),
                              w=[oacc.b], r=[po.b, zt.b])
                items = [(hl, sub) for hl in range(4) for sub in range(4)]
                wps = {}

                def wscores(ii):
                    hl, sub = items[ii]
                    qb = qt * 4 + sub
                    pa = kb.rot("psW", psW)
                    pb2 = kb.rot("psW", psW)
                    wps[ii] = (pa, pb2)
                    for m in range(5):
                        tgt = pa.t[:, m * 128:(m + 1) * 128] if m < 4 else pb2.t[:, 0:128]
                        tb = pa.b if m < 4 else pb2.b
                        kb.op("pe", lambda: nc.tensor.matmul(tgt, lhsT=kw.t[:, (qb + m) * 128:(qb + m + 1) * 128],
                                                             rhs=qg.t[:, hl, qb * 128:(qb + 1) * 128], start=True, stop=True),
                              w=[tb], r=[kw.b, qg.b])
                wscores(0)
                for ii, (hl, sub) in enumerate(items):
                    hh = 4 * g + hl
                    s8 = qt * 4 + sub
                    qb = s8
                    if ii + 1 < len(items):
                        wscores(ii + 1)
                    pa, pb2 = wps.pop(ii)
                    sbt = kb.rot("Sb", Sb)
                    kb.op("dve", lambda: nc.vector.tensor_tensor(out=sbt.t[:, 0:4, :], in0=pa.t[:, 0:512].rearrange("p (m q) -> p m q", m=4),
                                                                 in1=wbg.t[:, hl, 0:4, :], op=ALU.add), w=[sbt.b], r=[pa.b, wbg.b])
                    kb.op("dve", lambda: nc.vector.tensor_tensor(out=sbt.t[:, 4, :], in0=pb2.t[:, 0:128], in1=wbg.t[:, hl, 4, :], op=ALU.add),
                          w=[sbt.b], r=[pb2.b, wbg.b])
                    ptw = kb.rot("PTw", PTw)
                    for m in range(5):
                        kb.op("act", lambda: nc.scalar.activation(out=ptw.t[:, m, :], in_=sbt.t[:, m, :], func=AF.Exp,
                                                                  bias=halo_bias.t[:, qb + m:qb + m + 1]), w=[ptw.b], r=[sbt.b, halo_bias.b])
                    pw = kb.rot("psPW", psPW)
                    for m in range(5):
                        kb.op("pe", lambda: nc.tensor.matmul(pw.t[:, 0:129], lhsT=ptw.t[:, m, :], rhs=Vw.t[:, qb + m, :],
                                                             start=(m == 0), stop=(m == 4)), w=[pw.b], r=[ptw.b, Vw.b])
                    zt = fac(pw.t, pw.b, 128, s8, hh * 3 + 2, False)
                    kb.op("dve", lambda: nc.vector.scalar_tensor_tensor(out=oacc.t[:, sub, hl, :], in0=pw.t[:, 0:128], scalar=zt.t[:, 2:3],
                                                                        in1=oacc.t[:, sub, hl, :], op0=ALU.mult, op1=ALU.add),
                          w=[oacc.b], r=[pw.b, zt.b])
                sn = kb.rot("stN", stN)
                for hl in range(4):
                    p2 = kb.rot("psT", self.ps_misc)
                    for sub in range(4):
                        kb.op("pe", lambda: nc.tensor.transpose(out=p2.t[:, sub * 128:(sub + 1) * 128], in_=oacc.t[:, sub, hl, :],
                                                                identity=ident_f.t[:, :]), w=[p2.b], r=[oacc.b, ident_f.b])
                    self.evac("act" if hl % 2 else "dve", sn.t[:, hl, :], p2.t[:, 0:512], w=[sn.b], r=[p2.b])
                kb.dma("pool", [(nsaT_d[4 * g:4 * g + 4, :, q0:q0 + 512].rearrange("h p t -> p h t"), sn.t[:, :, :])], r=[sn.b])

    def resid_setup(self):
        kb = self.kb
        self.rs_f = [kb.tile("rsf", [128, 512], F32) for _ in range(2)]
        self.rs_x = [kb.tile("rsx", [128, 4, 128], F32) for _ in range(2)]
        self.rs_o = [kb.tile("rso", [128, 4, 128], F32) for _ in range(2)]

    def resid_epi(self, xsrc, xdst, pre=None):
        kb, nc = self.kb, self.nc

        def epi(ch, tt, ps, cw, n):
            s = kb.rot("rsf", self.rs_f)
            if pre is None:
                self.evac("act", s.t[:, :], ps.t[:, 0:512], w=[s.b], r=[ps.b])
            else:
                pre(ch, tt, ps, s)
            p2 = kb.rot("psT", self.ps_misc)
            for j in range(4):
                kb.op("pe", lambda: nc.tensor.transpose(out=p2.t[:, j * 128:(j + 1) * 128], in_=s.t[:, j * 128:(j + 1) * 128],
                                                        identity=self.ident_f.t[:, :]), w=[p2.b], r=[s.b, self.ident_f.b])
            xt = kb.rot("rsx", self.rs_x)
            kb.dma("sp", [(xt.t[:, :, :], xsrc[tt * 512:(tt + 1) * 512, ch * 128:(ch + 1) * 128].rearrange("(j p) c -> p j c", p=128))], w=[xt.b])
            xo = kb.rot("rso", self.rs_o)
            kb.op("dve", lambda: nc.vector.tensor_tensor(out=xo.t[:, :, :], in0=p2.t[:, 0:512].rearrange("p (j c) -> p j c", j=4),
                                                         in1=xt.t[:, :, :], op=ALU.add), w=[xo.b], r=[p2.b, xt.b])
            kb.dma("pool", [(xdst[tt * 512:(tt + 1) * 512, ch * 128:(ch + 1) * 128].rearrange("(j p) c -> p j c", p=128), xo.t[:, :, :])], r=[xo.b])
        return epi

    def phase_up_out(self):
        kb, nc, dr = self.kb, self.nc, self.dr
        kb.phase()
        self.common_consts()
        self.gemm_setup(3)
        self.resid_setup()
        x1 = self.scratch("x1", [NT, D], F32)
        actT = kb.tile("actT", [128, 16, NT], BF16)
        mres = kb.tile("mres", [128, 32, NT], BF16)
        gt = [kb.tile("gt", [128, 512], BF16) for _ in range(2)]
        tf = [kb.tile("tf", [128, 512], F32) for _ in range(2)]
        kb.dma("sp", [(actT.t[:, :, :], dr["poT_d"].rearrange("c p t -> p c t"))], w=[actT.b])

        def epi1(ch, tt, ps, cw, n):
            gg = kb.rot("gt", gt)
            kb.dma("sp", [(gg.t[:, :], dr["gpT"][ch * 128:(ch + 1) * 128, tt * 512:(tt + 1) * 512])], w=[gg.b])
            kb.op("dve", lambda: nc.vector.tensor_tensor(out=mres.t[:, ch, tt * 512:(tt + 1) * 512], in0=ps.t[:, 0:512], in1=gg.t[:, :], op=ALU.mult),
                  w=[mres.b], r=[ps.b, gg.b])
        self.gemm(dr["w_up_pool"], 2048, 0, D, NT, self.resident_act(actT), epi1)
        kb.dma("sp", [(actT.t[:, :, :], dr["nsaT_d"].rearrange("c p t -> p c t"))], w=[actT.b])

        def epi2(ch, tt, ps, cw, n):
            gg = kb.rot("gt", gt)
            kb.dma("sp", [(gg.t[:, :], dr["gnT"][ch * 128:(ch + 1) * 128, tt * 512:(tt + 1) * 512])], w=[gg.b])
            t = kb.rot("tf", tf)
            kb.op("dve", lambda: nc.vector.tensor_tensor(out=t.t[:, :], in0=ps.t[:, 0:512], in1=gg.t[:, :], op=ALU.mult),
                  w=[t.b], r=[ps.b, gg.b])
            o = mres.t[:, ch, tt * 512:(tt + 1) * 512]
            kb.op("pool", lambda: nc.gpsimd.tensor_tensor(out=o, in0=t.t[:, :], in1=o, op=ALU.add), w=[mres.b], r=[t.b])
        self.gemm(dr["w_up_nsa"], 2048, 0, D, NT, self.resident_act(actT), epi2)
        self.gemm(dr["w_out"], D, 0, D, NT, self.resident_act(mres), self.resid_epi(dr["x_ext"][NH:NE, :], x1))

    def phase_norm(self, xsrc, gname, dst_name):
        kb, nc, dr = self.kb, self.nc, self.dr
        kb.phase()
        self.common_consts()
        self.norm_setup()
        gfm = self.load_gfm(gname + "_bc")
        dst = self.scratch(dst_name, [32, 128, NT], BF16)
        hT = kb.tile("hT", [128, 32, NT], BF16)
        self.make_hT(xsrc, NT, gfm, hT)
        kb.dma("pool", [(dst.rearrange("c p t -> p c t"), hT.t[:, :, :])], r=[hT.b])

    def phase_peer_q(self):
        kb, nc, dr = self.kb, self.nc, self.dr
        kb.phase()
        self.common_consts()
        self.gemm_setup(3)
        qp_d = self.scratch("qp_d", [16, 128, NT], BF16)
        hT = kb.tile("hT", [128, 32, NT], BF16)
        kb.dma("sp", [(hT.t[:, :, :], dr["h2T_d"].rearrange("c p t -> p c t"))], w=[hT.b])
        stK = [kb.tile("stK", [128, 512], BF16) for _ in range(2)]

        def epi(ch, tt, ps, cw, n):
            s = kb.rot("stK", stK)
            self.evac("act", s.t[:, :], ps.t[:, 0:512], w=[s.b], r=[ps.b])
            kb.dma("pool", [(qp_d[ch, :, tt * 512:(tt + 1) * 512], s.t[:, :])], r=[s.b])
        self.gemm(dr["peer_w_q"], D, 0, 2048, NT, self.resident_act(hT), epi)

    def phase_peer_w(self):
        kb, nc, dr = self.kb, self.nc, self.dr
        kb.phase()
        self.common_consts()
        WT = self.scratch("WT_d", [16384, NT], BF16)
        WTv = WT.rearrange("(i j) t -> j i t", j=128)
        qp = kb.tile("qp", [128, 16, NT], BF16)
        kb.dma("sp", [(qp.t[:, :, :], dr["qp_d"].rearrange("c p t -> p c t"))], w=[qp.b])
        kf = self.const("keysT", [128, 2, 128], F32)
        kbf = kb.tile("kbf", [128, 2, 128], BF16)
        kb.op("dve", lambda: nc.vector.tensor_copy(out=kbf.t[:, :, :], in_=kf.t[:, :, :]), w=[kbf.b], r=[kf.b])
        S12s = [kb.tile("S12", [128, 8, 2, 128], F32) for _ in range(2)]
        TK = kb.tile("TK", [128, 8, 2, 16], F32)
        wk = [kb.tile("wk", [128, 128], F32) for _ in range(2)]
        cand = [kb.tile("cand", [128, 16, 16], F32) for _ in range(2)]
        cw2 = [kb.tile("cw2", [128, 256], F32) for _ in range(2)]
        cw3 = [kb.tile("cw3", [128, 256], F32) for _ in range(2)]
        vb = [kb.tile("vb", [128, 24], F32) for _ in range(2)]
        sc = [kb.tile("scl", [128, 16], F32) for _ in range(2)]
        e16 = [kb.tile("e16", [128, 16], F32) for _ in range(2)]
        s1p = [kb.tile("s1p", [128, 8, 128], F32) for _ in range(2)]
        LC = [kb.tile("LC", [128, 8], F32) for _ in range(2)]
        Tt = [kb.tile("Tt", [128, 16, 128], F32) for _ in range(3)]
        Et = [kb.tile("Et", [128, 16, 128], F32) for _ in range(3)]
        Mk = [kb.tile("Mk", [128, 16, 128], BF16) for _ in range(16)]
        WTs = [kb.tile("WTs", [128, 16, 128], BF16) for _ in range(2)]
        for t8 in range(8):
            S12 = kb.rot("S12", S12s)
            for hp in range(4):
                ps = kb.rot("acc", self.ps_acc)
                for hq in range(2):
                    h = hp * 2 + hq
                    for s in range(2):
                        kb.op("pe", lambda: nc.tensor.matmul(ps.t[:, (hq * 2 + s) * 128:(hq * 2 + s + 1) * 128],
                                                             lhsT=qp.t[:, 2 * h + s, t8 * 128:(t8 + 1) * 128], rhs=kbf.t[:, s, :],
                                                             start=True, stop=True), w=[ps.b], r=[qp.b, kbf.b])
                self.evac("act" if hp % 2 else "dve", S12.t[:, 2 * hp:2 * hp + 2, :, :].rearrange("p a s n -> p (a s n)"), ps.t[:, 0:512],
                          w=[S12.b], r=[ps.b])
            sp_ = kb.rot("s1p", s1p)
            lc = kb.rot("LC", LC)
            for h in range(8):
                for s in range(2):
                    w_ = kb.rot("wk", wk)
                    kb.op("dve", lambda: nc.vector.max(out=TK.t[:, h, s, 0:8], in_=S12.t[:, h, s, :]), w=[TK.b], r=[S12.b])
                    kb.op("dve", lambda: nc.vector.match_replace(out=w_.t[:, :], in_to_replace=TK.t[:, h, s, 0:8], in_values=S12.t[:, h, s, :],
                                                                 imm_value=-1e30), w=[w_.b], r=[TK.b, S12.b])
                    kb.op("dve", lambda: nc.vector.max(out=TK.t[:, h, s, 8:16], in_=w_.t[:, :]), w=[TK.b], r=[w_.b])
                cd = kb.rot("cand", cand)
                kb.op("dve", lambda: nc.vector.tensor_tensor(out=cd.t[:, :, :], in0=TK.t[:, h, 0, :].unsqueeze(2).to_broadcast([128, 16, 16]),
                                                             in1=TK.t[:, h, 1, :].unsqueeze(1).to_broadcast([128, 16, 16]), op=ALU.add),
                      w=[cd.b], r=[TK.b])
                cdf = cd.t[:, :, :].rearrange("p a b -> p (a b)")
                v = kb.rot("vb", vb)
                c2 = kb.rot("cw2", cw2)
                c3 = kb.rot("cw3", cw3)
                kb.op("dve", lambda: nc.vector.max(out=v.t[:, 0:8], in_=cdf), w=[v.b], r=[cd.b])
                kb.op("dve", lambda: nc.vector.match_replace(out=c2.t[:, :], in_to_replace=v.t[:, 0:8], in_values=cdf, imm_value=-1e30),
                      w=[c2.b], r=[v.b, cd.b])
                kb.op("dve", lambda: nc.vector.max(out=v.t[:, 8:16], in_=c2.t[:, :]), w=[v.b], r=[c2.b])
                x = kb.rot("scl", sc)
                e_ = kb.rot("e16", e16)
                kb.op("dve", lambda: nc.vector.tensor_scalar(out=x.t[:, 0:1], in0=v.t[:, 15:16], scalar1=-4e-6, scalar2=None, op0=ALU.add), w=[x.b], r=[v.b])
                kb.op("dve", lambda: nc.vector.tensor_scalar(out=x.t[:, 1:2], in0=v.t[:, 0:1], scalar1=-1.0, scalar2=None, op0=ALU.mult), w=[x.b], r=[v.b])
                kb.op("act", lambda: nc.scalar.activation(out=e_.t[:, :], in_=v.t[:, 0:16], func=AF.Exp, bias=x.t[:, 1:2]),
                      w=[e_.b], r=[v.b, x.b])
                kb.op("dve", lambda: nc.vector.tensor_reduce(out=x.t[:, 2:3], in_=e_.t[:, :], axis=mybir.AxisListType.X, op=ALU.add),
                      w=[x.b], r=[e_.b])
                kb.op("act", lambda: nc.scalar.activation(out=x.t[:, 3:4], in_=x.t[:, 2:3], func=AF.Ln), w=[x.b], r=[x.b])
                kb.op("dve", lambda: nc.vector.tensor_tensor(out=x.t[:, 4:5], in0=x.t[:, 0:1], in1=x.t[:, 1:2], op=ALU.add), w=[x.b], r=[x.b])
                kb.op("dve", lambda: nc.vector.tensor_tensor(out=lc.t[:, h:h + 1], in0=x.t[:, 4:5], in1=x.t[:, 3:4], op=ALU.subtract), w=[lc.b], r=[x.b])
                kb.op("dve", lambda: nc.vector.tensor_scalar(out=sp_.t[:, h, :], in0=S12.t[:, h, 0, :], scalar1=x.t[:, 0:1], scalar2=None,
                                                             op0=ALU.subtract), w=[sp_.b], r=[S12.b, x.b])
            for ic in range(8):
                accs = [kb.rot("accw", kb.ps) for _ in range(4)]
                mks = []
                for h in range(8):
                    tt_ = kb.rot("Tt", Tt)
                    et = kb.rot("Et", Et)
                    mk = kb.rot("Mk", Mk)
                    i0_ = sp_.t[:, h, ic * 16:(ic + 1) * 16].unsqueeze(2).to_broadcast([128, 16, 128])
                    i1_ = S12.t[:, h, 1, :].unsqueeze(1).to_broadcast([128, 16, 128])
                    if False:
                        kb.op("dve", lambda: nc.vector.tensor_tensor(out=tt_.t[:, :, :], in0=i0_, in1=i1_, op=ALU.add), w=[tt_.b], r=[sp_.b, S12.b])
                    else:
                        kb.op("pool", lambda: nc.gpsimd.tensor_tensor(out=tt_.t[:, :, :], in0=i0_, in1=i1_, op=ALU.add), w=[tt_.b], r=[sp_.b, S12.b])
                    kb.op("act", lambda: nc.scalar.activation(out=et.t[:, :, :], in_=tt_.t[:, :, :], func=AF.Exp, bias=lc.t[:, h:h + 1]),
                          w=[et.b], r=[tt_.b, lc.b])
                    kb.op("dve", lambda: nc.vector.scalar_tensor_tensor(out=mk.t[:, :, :], in0=tt_.t[:, :, :], scalar=0.0, in1=et.t[:, :, :],
                                                                        op0=ALU.is_ge, op1=ALU.mult), w=[mk.b], r=[tt_.b, et.b])
                    mks.append(mk)
                for i in range(16):
                    a = accs[i // 4]
                    for h in range(8):
                        mk = mks[h]
                        kb.op("pe", lambda: nc.tensor.matmul(a.t[:, (i % 4) * 128:(i % 4 + 1) * 128], lhsT=mk.t[:, i, :], rhs=self.ident_b.t[:, :],
                                                             start=(h == 0), stop=(h == 7)), w=[a.b], r=[mk.b, self.ident_b.b])
                ws = kb.rot("WTs", WTs)
                for b4 in range(4):
                    self.evac("act" if b4 % 2 else "dve", ws.t[:, b4 * 4:(b4 + 1) * 4, :].rearrange("p a t -> p (a t)"), accs[b4].t[:, 0:512],
                              w=[ws.b], r=[accs[b4].b])
                kb.dma("pool", [(WTv[:, ic * 16:(ic + 1) * 16, t8 * 128:(t8 + 1) * 128], ws.t[:, :, :])], r=[ws.b])

    def phase_peer_a(self):
        kb, nc, dr = self.kb, self.nc, self.dr
        kb.phase()
        self.common_consts()
        self.gemm_setup(3)
        coef = self.scratch("coefT_d", [16384, NT], BF16)
        hT = kb.tile("hT", [128, 32, NT], BF16)
        kb.dma("sp", [(hT.t[:, :, :], dr["h2T_d"].rearrange("c p t -> p c t"))], w=[hT.b])
        t1 = [kb.tile("g1", [128, 512], F32) for _ in range(2)]
        t2 = [kb.tile("g2", [128, 512], F32) for _ in range(2)]
        t3 = [kb.tile("g3", [128, 512], F32) for _ in range(2)]
        wt = [kb.tile("gw", [128, 512], BF16) for _ in range(2)]
        cf = [kb.tile("gc", [128, 512], BF16) for _ in range(2)]

        def epi(ch, tt, ps, cw, n):
            a1, a2, a3, w_, c_ = kb.rot("g1", t1), kb.rot("g2", t2), kb.rot("g3", t3), kb.rot("gw", wt), kb.rot("gc", cf)
            kb.dma("sp", [(w_.t[:, :], dr["WT_d"][ch * 128:(ch + 1) * 128, tt * 512:(tt + 1) * 512])], w=[w_.b])
            kb.op("act", lambda: nc.scalar.activation(out=a1.t[:, :], in_=ps.t[:, 0:512], func=AF.Square), w=[a1.b], r=[ps.b])
            kb.op("dve", lambda: nc.vector.tensor_scalar(out=a1.t[:, :], in0=a1.t[:, :], scalar1=0.044715, scalar2=1.0, op0=ALU.mult, op1=ALU.add),
                  w=[a1.b], r=[a1.b])
            kb.op("dve", lambda: nc.vector.tensor_tensor(out=a2.t[:, :], in0=a1.t[:, :], in1=ps.t[:, 0:512], op=ALU.mult), w=[a2.b], r=[a1.b, ps.b])
            kb.op("act", lambda: nc.scalar.activation(out=a3.t[:, :], in_=a2.t[:, :], func=AF.Sigmoid, scale=1.5957691216057308),
                  w=[a3.b], r=[a2.b])
            kb.op("dve", lambda: nc.vector.tensor_tensor(out=a3.t[:, :], in0=a3.t[:, :], in1=ps.t[:, 0:512], op=ALU.mult), w=[a3.b], r=[a3.b, ps.b])
            kb.op("pool", lambda: nc.gpsimd.tensor_tensor(out=c_.t[:, :], in0=a3.t[:, :], in1=w_.t[:, :], op=ALU.mult), w=[c_.b], r=[a3.b, w_.b])
            kb.dma("pool", [(coef[ch * 128:(ch + 1) * 128, tt * 512:(tt + 1) * 512], c_.t[:, :])], r=[c_.b])
        self.gemm(dr["peer_uT"], D, 0, 16384, NT, self.resident_act(hT), epi)

    def phase_peer_o(self):
        kb, nc, dr = self.kb, self.nc, self.dr
        kb.phase()
        self.common_consts()
        self.gemm_setup(3)
        self.resid_setup()
        x2 = self.scratch("x2", [NT, D], F32)
        cT = [kb.tile("cT", [128, 16, NT], BF16) for _ in range(3)]
        cv = dr["coefT_d"].rearrange("(k p) t -> p k t", p=128)

        def act_fn(gi):
            t = kb.rot("cT", cT)
            kb.dma("sp", [(t.t[:, :, :], cv[:, gi * 16:(gi + 1) * 16, :])], w=[t.b])

            def ap(kl, t0, n):
                return t.t[:, kl, t0:t0 + n]
            return ap, t.b
        self.gemm(dr["peer_v"], 16384, 0, D, NT, act_fn, self.resid_epi(dr["x1"], x2))

    def phase_ple(self):
        kb, nc, dr = self.kb, self.nc, self.dr
        kb.phase()
        self.common_consts()
        self.gemm_setup(3)
        self.resid_setup()
        projT = self.scratch("projT_d", [D, NT], F32)
        x3 = self.scratch("x3", [NT, D], F32)
        pf = kb.tile("pf", [128, 2, NT], F32)
        pb = kb.tile("pb", [128, 2, NT], BF16)
        kb.dma("sp", [(pf.t[:, :, :], dr["pT"].rearrange("(k p) t -> p k t", p=128))], w=[pf.b])
        kb.op("dve", lambda: nc.vector.tensor_copy(out=pb.t[:, :, :], in_=pf.t[:, :, :]), w=[pb.b], r=[pf.b])
        stF = [kb.tile("stF", [128, 512], F32) for _ in range(2)]

        def epi_p(ch, tt, ps, cw, n):
            s = kb.rot("stF", stF)
            self.evac("act", s.t[:, :], ps.t[:, 0:512], w=[s.b], r=[ps.b])
            kb.dma("pool", [(projT[ch * 128:(ch + 1) * 128, tt * 512:(tt + 1) * 512], s.t[:, :])], r=[s.b])
        self.gemm(dr["ple_w_proj"], 256, 0, D, NT, self.resident_act(pb), epi_p)
        kb.barrier()
        rT = kb.tile("rT", [128, 32, NT], BF16)
        kb.dma("sp", [(rT.t[:, :, :], dr["rT_d"].rearrange("c p t -> p c t"))], w=[rT.b])
        pj = [kb.tile("pj", [128, 512], F32) for _ in range(2)]
        sg = [kb.tile("sg", [128, 512], F32) for _ in range(2)]

        def pre(ch, tt, ps, s):
            p_ = kb.rot("pj", pj)
            g_ = kb.rot("sg", sg)
            kb.dma("sp", [(p_.t[:, :], projT[ch * 128:(ch + 1) * 128, tt * 512:(tt + 1) * 512])], w=[p_.b])
            kb.op("act", lambda: nc.scalar.activation(out=g_.t[:, :], in_=ps.t[:, 0:512], func=AF.Sigmoid), w=[g_.b], r=[ps.b])
            kb.op("dve", lambda: nc.vector.tensor_tensor(out=s.t[:, :], in0=g_.t[:, :], in1=p_.t[:, :], op=ALU.mult), w=[s.b], r=[g_.b, p_.b])
        self.gemm(dr["ple_w_gate"], D, 0, D, NT, self.resident_act(rT), self.resid_epi(dr["x2"], x3, pre=pre))

    def phase_final(self):
        kb, nc, dr = self.kb, self.nc, self.dr
        kb.phase()
        self.norm_setup()
        gbc = kb.tile("gbc", [128, D], F32)
        kb.dma("sp", [(gbc.t[:, :], dr["g_fin"][0:1, :].to_broadcast([128, D]))], w=[gbc.b])
        for tt in range(NT // 128):
            xt = kb.rot("xb", self.xb)
            s = kb.rot("st", self.st)
            kb.dma("sp", [(xt.t[:, :], dr["x3"][tt * 128:(tt + 1) * 128, :])], w=[xt.b])
            self.row_rstd(xt, s)
            kb.op("dve", lambda: nc.vector.scalar_tensor_tensor(out=xt.t[:, :], in0=xt.t[:, :], scalar=s.t[:, 11:12], in1=gbc.t[:, :],
                                                                op0=ALU.mult, op1=ALU.mult), w=[xt.b], r=[s.b, gbc.b])
            kb.dma("pool", [(dr["y"][tt * 128:(tt + 1) * 128, :], xt.t[:, :])], r=[xt.b])

    PHASES = ["kv_all", "own", "poolg", "cmp", "att", "up_out", "norm2", "peer_q", "peer_w", "peer_a", "peer_o", "norm3", "ple", "final"]

    def build(self):
        dr = self.dr
        for ph in self.PHASES:
            if ph == "norm2":
                self.phase_norm(dr["x1"], "g_ffn", "h2T_d")
            elif ph == "norm3":
                self.phase_norm(dr["x2"], "g_ple", "rT_d")
            else:
                getattr(self, "phase_" + ph)()
            if self.stop_after == ph:
                break
        self.kb.barrier()
        for name, shape, dt in self.debug:
            self.kb.barrier()
            ap = self.dbg_out(name, shape, dt)
            self.kb.dma("sp", [(ap, self.dr[name])])
        self.kb.barrier()
        return self.nc


def prep_shared(inp):
    f = lambda a: np.ascontiguousarray(np.asarray(a, dtype=np.float32))
    sh = {}
    sh["x_all"] = f(inp["x"][0])
    for k, n in (("g_mix", "norm_mix_g"), ("g_ffn", "norm_ffn_g"), ("g_ple", "norm_ple_g")):
        sh[k + "_bc"] = f(inp[n][0]).reshape(1, D)
    sh["g_fin"] = f(inp["norm_final_g"]).reshape(1, D)
    def pan(w, pw=256):
        w = np.asarray(w, np.float32)
        K, N = w.shape
        return np.ascontiguousarray(w.reshape(K // 128, 128, N // pw, pw).transpose(2, 1, 0, 3))
    sh["_pan"] = pan
    w_in = np.asarray(inp["w_in"][0], np.float32)
    sh["w_in_a"] = pan(w_in[:, 0:7168])
    sh["w_in_g"] = pan(w_in[:, 7168:7216], 48)
    sh["w_in_b"] = pan(w_in[:, 7216:])
    pw_ = np.asarray(inp["pool_w"][0], np.float32)
    sh["pool_w"] = np.ascontiguousarray(np.concatenate([pan(pw_[g]) for g in range(4)], axis=0))
    sh["pool_scale"] = f(f(inp["pool_scale"][0]).reshape(16, 128).T)
    sh["posT"] = f(np.stack([f(inp["cmp_pos_k"][0]).T, f(inp["cmp_pos_v"][0]).T], axis=1))
    sh["cmp_w_k"] = f(inp["cmp_w_k"][0])
    sh["cmp_w_v"] = f(inp["cmp_w_v"][0])
    sh["w_up_pool"] = pan(inp["w_up_pool"][0])
    sh["w_up_nsa"] = pan(inp["w_up_nsa"][0])
    sh["w_out"] = pan(inp["w_out"][0])
    sh["peer_w_q"] = pan(inp["peer_w_q"][0])
    sh["keysT"] = f(np.stack([f(inp["peer_keys1"][0]).T, f(inp["peer_keys2"][0]).T], axis=1))
    sh["ple_w_gate"] = pan(inp["ple_w_gate"][0])
    sh["ple_w_proj"] = pan(inp["ple_w_proj"][0])
    sh["_peer_u"] = inp["peer_u"]
    sh["_peer_v"] = inp["peer_v"]
    sh["_p"] = inp["p"]
    sh.update(static_tables())
    return sh


def core_inputs(sh, c, used):
    m = {}
    x = sh["x_all"]
    for name in used:
        if name == "x_ext":
            xe = np.zeros((NE, D), np.float32)
            lo = c * NT - NH
            if lo >= 0:
                xe[:] = x[lo:lo + NE]
            else:
                xe[NH:] = x[0:NT]
            m[name] = xe
        elif name == "pT":
            m[name] = np.ascontiguousarray(np.asarray(sh["_p"][0, 0, c * NT:(c + 1) * NT, :], np.float32).T)
        elif name == "peer_uT":
            if "peer_uT" not in sh:
                sh["peer_uT"] = sh["_pan"](np.asarray(sh["_peer_u"][0], np.float32).T)
            m[name] = sh["peer_uT"]
        elif name == "peer_v":
            if "peer_v" not in sh:
                sh["peer_v"] = sh["_pan"](np.asarray(sh["_peer_v"][0], np.float32))
            m[name] = sh["peer_v"]
        elif name in sh:
            m[name] = sh[name]
        else:
            if ("_core", c) not in sh:
                sh[("_core", c)] = core_tables(c)
            m[name] = sh[("_core", c)][name]
    return m


_CACHE = {}


def kernel(**inputs):
    if "prog" not in _CACHE:
        P = Prog()
        P.build()
        _CACHE["prog"] = P
    P = _CACHE["prog"]
    sh = prep_shared(inputs)
    in_maps = [core_inputs(sh, c, P.used_inputs) for c in range(NCORES)]
    res = run_bass_kernel_spmd(P.nc, in_maps, core_ids=list(range(NCORES)))
    out = np.concatenate([np.asarray(res.results[c]["y"], np.float32) for c in range(NCORES)], axis=0)
    return out.reshape(1, T, D)
```

```python
import math
from contextlib import ExitStack

import numpy as np
import ml_dtypes

import concourse.bass as bass
import concourse.mybir as mybir
from concourse.bass_utils import run_bass_kernel_spmd

F32 = mybir.dt.float32
BF16 = mybir.dt.bfloat16
AF = mybir.ActivationFunctionType
ALU = mybir.AluOpType
NPBF = ml_dtypes.bfloat16

NCORES = 8
T = 8192
D = 4096
NT = 1024
NH = 512
NE = NT + NH
INW = 15408
EPS = 1e-6
BIG = 32768.0
POOL_WINDOWS = (2, 4, 8, 16)
SLOPES = (2.0 ** (-8.0 * np.arange(1, 17) / 16)).astype(np.float32)
SAME_ENGINE_SYNC = True


class Buf:
    __slots__ = ("w", "r")

    def __init__(self):
        self.w = {}
        self.r = {}


class Tile:
    def __init__(self, kb, name, shape, dtype, space="sb", stack=None):
        nc = kb.nc
        cm = nc.sbuf_tensor(name, shape, dtype) if space == "sb" else nc.psum_tensor(name, shape, dtype)
        self.t = (stack if stack is not None else kb.stack).enter_context(cm)
        self.b = Buf()


class KB:
    ENG = ("pe", "act", "dve", "pool", "sp")

    def __init__(self):
        self.nc = bass.Bass("TRN2", target_bir_lowering=False)
        nc = self.nc
        self.gstack = ExitStack()
        self.stack = self.gstack
        self.e = {"pe": nc.tensor, "act": nc.scalar, "dve": nc.vector, "pool": nc.gpsimd, "sp": nc.sync}
        self.semh = {}
        self.cnt = {}
        for en in self.ENG:
            self.semh["e_" + en] = self.gstack.enter_context(nc.semaphore("sem_" + en))
            self.cnt["e_" + en] = 0
        self.ND = 40
        for i in range(self.ND):
            self.semh["d_%d" % i] = self.gstack.enter_context(nc.semaphore("semd_%d" % i))
            self.cnt["d_%d" % i] = 0
        self.dma_rr = 0
        self.seen = {en: {} for en in self.ENG}
        self.uid = 0
        self.ps = [Tile(self, "psb%d" % i, [128, 512], F32, "ps", self.gstack) for i in range(8)]
        self.rr = {}

    def name(self, s):
        self.uid += 1
        return "%s_%d" % (s, self.uid)

    def tile(self, name, shape, dtype):
        return Tile(self, self.name(name), shape, dtype, "sb")

    def rot(self, key, lst):
        i = self.rr.get(key, 0)
        self.rr[key] = i + 1
        return lst[i % len(lst)]

    def _waits(self, eng, w, r):
        waits = {}
        for b in r:
            for s, v in b.w.items():
                if waits.get(s, 0) < v:
                    waits[s] = v
        for b in w:
            for dct in (b.w, b.r):
                for s, v in dct.items():
                    if waits.get(s, 0) < v:
                        waits[s] = v
        own = "e_" + eng
        E = self.e[eng]
        seen = self.seen[eng]
        for s, v in waits.items():
            if s == own and (eng == "pe" or not SAME_ENGINE_SYNC):
                continue
            if seen.get(s, 0) >= v:
                continue
            E.wait_ge(self.semh[s], v)
            seen[s] = v

    def _record(self, tag, w, r):
        s, v = tag
        for b in r:
            if b.r.get(s, 0) < v:
                b.r[s] = v
        for b in w:
            b.w = {s: v}
            b.r = {}

    def op(self, eng, fn, w=(), r=()):
        self._waits(eng, w, r)
        inst = fn()
        s = "e_" + eng
        self.cnt[s] += 1
        inst.then_inc(self.semh[s], 1)
        self._record((s, self.cnt[s]), w, r)

    def dma(self, q, pairs, w=(), r=()):
        s = "d_%d" % (self.dma_rr % self.ND)
        self.dma_rr += 1
        E = self.e[q]
        seen = self.seen[q]
        prev = self.cnt[s]
        if prev > 0 and seen.get(s, 0) < prev:
            E.wait_ge(self.semh[s], prev)
            seen[s] = prev
        self._waits(q, w, r)
        for (o, i) in pairs:
            E.dma_start(out=o, in_=i).then_inc(self.semh[s], 16)
            self.cnt[s] += 16
        self._record((s, self.cnt[s]), w, r)

    def barrier(self):
        for en in self.ENG:
            E = self.e[en]
            seen = self.seen[en]
            for s, v in self.cnt.items():
                if v == 0 or s == "e_" + en:
                    continue
                if seen.get(s, 0) >= v:
                    continue
                E.wait_ge(self.semh[s], v)
                seen[s] = v

    def push(self):
        self._saved = self.stack
        self.stack = ExitStack()

    def pop(self):
        self.barrier()
        self.stack.close()
        self.stack = self._saved

    def phase(self):
        self.barrier()
        if self.stack is not self.gstack:
            self.stack.close()
        self.stack = ExitStack()
        self.rr = {}


def hml(v):
    v = np.asarray(v, np.float32)
    hi = v.astype(NPBF)
    r1 = v - hi.astype(np.float32)
    mid = r1.astype(NPBF)
    r2 = r1 - mid.astype(np.float32)
    lo = r2.astype(NPBF)
    return np.stack([hi, mid, lo], 0)


def static_tables():
    k = np.arange(128)
    tb = {}
    tb["ident_f"] = np.eye(128, dtype=np.float32)
    tb["ident_b"] = np.eye(128, dtype=np.float32).astype(NPBF)
    tb["ones3"] = np.ones((3, 128), np.float32).astype(NPBF)
    j = np.arange(512, dtype=np.float32)
    aq = np.zeros((3, 16, 512), NPBF)
    for h in range(16):
        aq[:, h, :] = hml(-SLOPES[h] * j)
    tb["aq"] = aq
    n = np.arange(512)
    c_start = n * 16
    s_start = np.arange(128) * 64
    ov = ((c_start[:, None] < s_start[None, :] + 64) & (c_start[:, None] + 32 > s_start[None, :])).astype(np.float32)
    ov[511, :] = 0.0
    rc = np.zeros((512, 129), np.float32)
    rc[:, :128] = ov
    rc[:511, 128] = 1.0
    tb["ovl1"] = rc.reshape(4, 128, 129).transpose(1, 0, 2).astype(NPBF).copy()
    eh = np.zeros((128, 56, 128), np.float32)
    for jj in range(56):
        eh[2 * jj, jj, :64] = 1.0
        eh[2 * jj + 1, jj, 64:] = 1.0
    eh[112:115, :, :] = 1.0
    tb["ehx"] = eh.astype(NPBF)
    eo = np.zeros((19, 8, 128), np.float32)
    for jj in range(8):
        eo[2 * jj, jj, :64] = 1.0
        eo[2 * jj + 1, jj, 64:] = 1.0
    eo[16:19, :, :] = 1.0
    tb["eox"] = eo.astype(NPBF)
    q = np.arange(512)
    cd = np.zeros((128, 4, 512), np.float32)
    for r in range(4):
        cd[:, r, :] = np.where((128 * r + k)[:, None] > q[None, :], -BIG, 0.0)
    tb["cdiag"] = cd.astype(NPBF)
    kbo = np.zeros((128, 16, 2, 8), np.float32)
    for h in range(16):
        for qt in range(2):
            for jo in range(8):
                kbo[:, h, qt, jo] = SLOPES[h] * (128 * jo + k - 512 * qt)
    tb["kb_o"] = kbo
    wb = np.zeros((128, 16, 5, 128), np.float32)
    qq = np.arange(128)
    for m in range(5):
        d = 128 * (4 - m) + (qq[None, :] - k[:, None])
        ok = (d >= 0) & (d < 512)
        for h in range(16):
            wb[:, h, m, :] = np.where(ok, -SLOPES[h] * d, -30000.0)
    tb["wbias"] = wb
    return tb


def core_tables(c):
    k = np.arange(128)
    tb = {}
    base = 1024 * c
    tq = base + np.arange(1024)
    cm = np.zeros((128, 4, 1024), np.float32)
    for ch in range(4):
        nn = 128 * ch + k
        end = 16 * nn + 31
        bad = (end[:, None] > tq[None, :]) | (nn[:, None] >= 511)
        cm[:, ch, :] = np.where(bad, -BIG, 0.0)
    tb["cmask"] = cm.astype(NPBF)
    kbc = np.zeros((128, 16, 2, 4), np.float32)
    kbh = np.zeros((128, 16, 2, 56), np.float32)
    for h in range(16):
        for qt in range(2):
            tref = base + 512 * qt
            for ch in range(4):
                kbc[:, h, qt, ch] = SLOPES[h] * (16 * (128 * ch + k) + 31 - tref)
            for jj in range(56):
                kbh[:, h, qt, jj] = SLOPES[h] * (128 * jj + k - tref)
    tb["kb_c"] = kbc
    tb["kb_h"] = kbh
    cur = (tq // 64)
    kk = np.arange(128)
    valid = (kk[None, :] <= cur[:, None]).astype(np.float32)
    forced = ((kk[None, :] == 0) | (kk[None, :] == cur[:, None]) | (kk[None, :] == cur[:, None] - 1)).astype(np.float32)
    addt = valid - 1.0 + 1e4 * forced
    tb["valid_t"] = valid.reshape(8, 128, 128).transpose(1, 0, 2).copy()
    tb["addt"] = addt.reshape(8, 128, 128).transpose(1, 0, 2).copy()
    hc = np.where((kk < 16 * c) & (kk < 112), 0.0, -BIG).astype(np.float32)
    tb["histcap"] = hc.reshape(128, 1)
    pc = np.zeros((128, 16), np.float32)
    for b in range(16):
        pc[16 * c + b, b] = 1.0
    tb["pc"] = pc.astype(NPBF)
    hb = np.zeros((128, 12), np.float32)
    if c == 0:
        hb[:, 0:4] = -30000.0
    tb["halo_bias"] = hb
    ic = np.zeros((4, 1024), np.float32)
    for gi, w in enumerate(POOL_WINDOWS):
        ic[gi] = 1.0 / np.minimum(tq + 1.0, float(w))
    tb["invcnt"] = ic
    return tb


STATIC_SPECS = [("ident_f", [128, 128], F32), ("ident_b", [128, 128], BF16), ("ones3", [3, 128], BF16),
                ("aq", [3, 16, 512], BF16), ("ovl1", [128, 4, 129], BF16), ("ehx", [128, 56, 128], BF16),
                ("eox", [19, 8, 128], BF16), ("cdiag", [128, 4, 512], BF16), ("kb_o", [128, 16, 2, 8], F32),
                ("wbias", [128, 16, 5, 128], F32)]
CORE_SPECS = [("cmask", [128, 4, 1024], BF16), ("kb_c", [128, 16, 2, 4], F32), ("kb_h", [128, 16, 2, 56], F32),
              ("valid_t", [128, 8, 128], F32), ("addt", [128, 8, 128], F32), ("histcap", [128, 1], F32),
              ("pc", [128, 16], BF16), ("halo_bias", [128, 12], F32), ("invcnt", [4, 1024], F32)]
WEIGHT_SPECS = [("x_all", [T, D]), ("x_ext", [NE, D]), ("pT", [256, NT]),
                ("g_mix_bc", [1, D]), ("g_ffn_bc", [1, D]), ("g_ple_bc", [1, D]), ("g_fin", [1, D]),
                ("w_in_a", [28, 128, 32, 256]), ("w_in_g", [1, 128, 32, 48]), ("w_in_b", [32, 128, 32, 256]), ("pool_w", [8, 128, 4, 256]), ("pool_scale", [128, 16]),
                ("posT", [128, 2, 32]), ("cmp_w_k", [4096, 128]), ("cmp_w_v", [4096, 128]),
                ("w_up_pool", [16, 128, 16, 256]), ("w_up_nsa", [16, 128, 16, 256]), ("w_out", [16, 128, 32, 256]),
                ("peer_w_q", [8, 128, 32, 256]), ("keysT", [128, 2, 128]), ("peer_uT", [64, 128, 32, 256]), ("peer_v", [16, 128, 128, 256]),
                ("ple_w_gate", [16, 128, 32, 256]), ("ple_w_proj", [16, 128, 2, 256])]


class Prog:
    def __init__(self, stop_after=None, debug=()):
        self.kb = KB()
        self.nc = self.kb.nc
        self.stop_after = stop_after
        self.debug = list(debug)
        self.dbg_specs = []
        nc = self.nc
        specs = {name: (shape, F32) for name, shape in WEIGHT_SPECS}
        specs.update({name: (shape, dt) for name, shape, dt in STATIC_SPECS + CORE_SPECS})
        prog = self

        class Lazy(dict):
            def __missing__(self, name):
                shape, dt = specs[name]
                ap = nc.dram_tensor(name, shape, dt, kind="ExternalInput").ap()
                self[name] = ap
                prog.used_inputs.append(name)
                return ap
        self.used_inputs = []
        self.dr = Lazy()
        self.dr["y"] = nc.dram_tensor("y", [NT, D], F32, kind="ExternalOutput").ap()

    def scratch(self, name, shape, dt):
        self.dr[name] = self.nc.dram_tensor(name, shape, dt, kind="Internal").ap()
        return self.dr[name]

    def dbg_out(self, name, shape, dt=F32):
        ap = self.nc.dram_tensor("dbg_" + name, shape, dt, kind="ExternalOutput").ap()
        self.dbg_specs.append("dbg_" + name)
        return ap

    def const(self, name, shape, dt, src=None, q="sp"):
        kb = self.kb
        t = kb.tile(name, shape, dt)
        s = self.dr[name] if src is None else src
        kb.dma(q, [(t.t[tuple(slice(None) for _ in shape)], s)], w=[t.b])
        return t

    def load_gfm(self, gname):
        kb = self.kb
        t = kb.tile("gbc", [128, D], F32)
        kb.dma("sp", [(t.t[:, :], self.dr[gname][0:1, :].to_broadcast([128, D]))], w=[t.b])
        return t

    def norm_setup(self):
        kb = self.kb
        self.xb = [kb.tile("xb", [128, D], F32) for _ in range(2)]
        self.xsb = [kb.tile("xsb", [128, D], BF16) for _ in range(2)]
        self.scr = [kb.tile("scr", [128, 1024], F32) for _ in range(2)]
        self.st = [kb.tile("st", [128, 12], F32) for _ in range(2)]

    def row_rstd(self, xt, s):
        kb, nc = self.kb, self.nc
        for i in range(4):
            scr = kb.rot("scr", self.scr)
            kb.op("act", lambda: nc.scalar.activation(out=scr.t[:, :], in_=xt.t[:, i * 1024:(i + 1) * 1024], func=AF.Square),
                  w=[scr.b], r=[xt.b])
            kb.op("dve", lambda: nc.vector.tensor_reduce(out=s.t[:, i:i + 1], in_=scr.t[:, :], axis=mybir.AxisListType.X, op=ALU.add),
                  w=[s.b], r=[scr.b])
        kb.op("dve", lambda: nc.vector.tensor_reduce(out=s.t[:, 6:7], in_=s.t[:, 0:4], axis=mybir.AxisListType.X, op=ALU.add), w=[s.b], r=[s.b])
        kb.op("dve", lambda: nc.vector.tensor_scalar(out=s.t[:, 7:8], in0=s.t[:, 6:7], scalar1=1.0 / D, scalar2=EPS,
                                                     op0=ALU.mult, op1=ALU.add), w=[s.b], r=[s.b])
        kb.op("act", lambda: nc.scalar.sqrt(out=s.t[:, 8:9], in_=s.t[:, 7:8]), w=[s.b], r=[s.b])
        kb.op("dve", lambda: nc.vector.reciprocal(out=s.t[:, 11:12], in_=s.t[:, 8:9]), w=[s.b], r=[s.b])

    def make_hT(self, xsrc, ntok, gbc, hT, tok_off=0):
        for tt in range(ntok // 128):
            self.make_hT_tile(xsrc, tt, gbc, hT, tok_off)

    def make_hT_tile(self, xsrc, tt, gbc, hT, tok_off=0):
        xs = self.make_hT_prep(xsrc, tt, gbc)
        self.make_hT_xpose(xs, tt, hT, tok_off)

    def make_hT_prep(self, xsrc, tt, gbc):
        kb, nc = self.kb, self.nc
        xt = kb.rot("xb", self.xb)
        xs = kb.rot("xsb", self.xsb)
        s = kb.rot("st", self.st)
        kb.dma("sp", [(xt.t[:, :], xsrc[tt * 128:(tt + 1) * 128, :])], w=[xt.b])
        self.row_rstd(xt, s)
        kb.op("dve", lambda: nc.vector.scalar_tensor_tensor(out=xs.t[:, :], in0=xt.t[:, :], scalar=s.t[:, 11:12], in1=gbc.t[:, :],
                                                            op0=ALU.mult, op1=ALU.mult), w=[xs.b], r=[xt.b, s.b, gbc.b])
        return xs

    def make_hT_xpose(self, xs, tt, hT, tok_off=0):
        kb, nc = self.kb, self.nc
        ident = self.ident_b
        for b in range(4):
            ps = kb.rot("psT", self.ps_misc)
            pv = ps.t[:, 0:512].bitcast(BF16)
            for j in range(8):
                kc = b * 8 + j
                kb.op("pe", lambda: nc.tensor.transpose(out=pv[:, j * 128:(j + 1) * 128],
                                                        in_=xs.t[:, kc * 128:(kc + 1) * 128], identity=ident.t[:, :]),
                      w=[ps.b], r=[xs.b, ident.b])
            o = hT.t[:, b * 8:(b + 1) * 8, tok_off + tt * 128: tok_off + (tt + 1) * 128]
            i = pv[:, 0:1024].rearrange("p (j t) -> p j t", j=8)
            self.evac("dve" if b % 2 == 0 else "act", o, i, w=[hT.b], r=[ps.b])

    def gemm_setup(self, nbuf=2):
        kb = self.kb
        self.wf = [kb.tile("wf", [128, 16, 256], F32) for _ in range(nbuf)]
        self.wb = [kb.tile("wb", [128, 16, 256], BF16) for _ in range(nbuf)]
        self.pf_dist = nbuf - 1

    def gemm(self, W, K, col0, ncols, ntok, act_fn, epi, banks=None, hook=None):
        kb, nc = self.kb, self.nc
        banks = banks if banks is not None else self.ps_acc
        nkc = K // 128
        kgs = [(k0, min(16, nkc - k0)) for k0 in range(0, nkc, 16)]
        ntt = (ntok + 511) // 512
        npan = (ncols + 255) // 256
        blocks = [(p, gi) for p in range(npan) for gi in range(len(kgs))]
        ready = {}

        def prefetch(bi):
            p, gi = blocks[bi]
            k0, nk = kgs[gi]
            pw = min(256, ncols - p * 256)
            wf = kb.rot("wf", self.wf)
            wb = kb.rot("wb", self.wb)
            kb.dma("sp", [(wf.t[:, 0:nk, 0:pw], W[col0 // 256 + p, :, k0:k0 + nk, 0:pw])], w=[wf.b])
            ce = kb.rot("cast", ["pool", "dve", "act"])
            o, i = wb.t[:, 0:nk, 0:pw], wf.t[:, 0:nk, 0:pw]
            if ce == "pool":
                kb.op("pool", lambda: nc.gpsimd.tensor_copy(out=o, in_=i), w=[wb.b], r=[wf.b])
            elif ce == "dve":
                kb.op("dve", lambda: nc.vector.tensor_copy(out=o, in_=i), w=[wb.b], r=[wf.b])
            else:
                kb.op("act", lambda: nc.scalar.copy(out=o, in_=i), w=[wb.b], r=[wf.b])
            ready[bi] = (wb, act_fn(gi))
        dist = self.pf_dist
        for b0 in range(min(dist, len(blocks))):
            prefetch(b0)
        accs = {}
        for bi, (p, gi) in enumerate(blocks):
            k0, nk = kgs[gi]
            pw = min(256, ncols - p * 256)
            nch = (pw + 127) // 128
            if gi == 0:
                if hook is not None:
                    hook(p)
                accs = {}
                for c in range(nch):
                    for tt in range(ntt):
                        accs[(c, tt)] = kb.rot("acc", banks)
            if bi + dist < len(blocks):
                prefetch(bi + dist)
            wb, (act_ap, act_b) = ready.pop(bi)
            for c in range(nch):
                cw = min(128, pw - c * 128)
                for tt in range(ntt):
                    n = min(512, ntok - tt * 512)
                    ps = accs[(c, tt)]
                    for kl in range(nk):
                        st_ = (gi == 0 and kl == 0)
                        sp_ = (gi == len(kgs) - 1 and kl == nk - 1)
                        kb.op("pe", lambda: nc.tensor.matmul(ps.t[0:cw, 0:n], lhsT=wb.t[:, kl, c * 128:c * 128 + cw],
                                                             rhs=act_ap(kl, tt * 512, n), start=st_, stop=sp_),
                              w=[ps.b], r=[wb.b, act_b])
            if gi == len(kgs) - 1:
                for c in range(nch):
                    cw = min(128, pw - c * 128)
                    for tt in range(ntt):
                        n = min(512, ntok - tt * 512)
                        epi(p * 2 + c, tt, accs[(c, tt)], cw, n)

    def resident_act(self, tile, kc0=0):
        def f(gi):
            def ap(kl, t0, n):
                return tile.t[:, kc0 + gi * 16 + kl, t0:t0 + n]
            return ap, tile.b
        return f

    def common_consts(self):
        kb = self.kb
        self.ps_misc = kb.ps[6:8]
        self.ps_acc = kb.ps[0:6]
        self.ident_f = self.const("ident_f", [128, 128], F32)
        self.ident_b = self.const("ident_b", [128, 128], BF16)

    def evac(self, eng, o, i, w, r, scale=None):
        kb, nc = self.kb, self.nc
        if eng == "dve":
            if scale is None:
                kb.op("dve", lambda: nc.vector.tensor_copy(out=o, in_=i), w=w, r=r)
            else:
                kb.op("dve", lambda: nc.vector.tensor_scalar(out=o, in0=i, scalar1=scale, scalar2=None, op0=ALU.mult), w=w, r=r)
        else:
            if scale is None:
                kb.op("act", lambda: nc.scalar.copy(out=o, in_=i), w=w, r=r)
            else:
                kb.op("dve", lambda: nc.vector.tensor_scalar(out=o, in0=i, scalar1=scale, scalar2=None, op0=ALU.mult), w=w, r=r)

    def fm_to_tm_bf16(self, src, n, dst_fn, dst_b):
        kb, nc = self.kb, self.nc
        ps = kb.rot("psT", self.ps_misc)
        nsub = n // 128
        pv = ps.t[:, 0:256].bitcast(BF16)
        for j in range(nsub):
            kb.op("pe", lambda: nc.tensor.transpose(out=pv[:, j * 128:(j + 1) * 128], in_=src.t[:, j * 128:(j + 1) * 128],
                                                    identity=self.ident_b.t[:, :]), w=[ps.b], r=[src.b, self.ident_b.b])
        for j in range(nsub):
            self.evac("dve" if j % 2 == 0 else "act", dst_fn(j), pv[:, j * 128:(j + 1) * 128], w=[dst_b], r=[ps.b])

    def phase_kv_all(self):
        kb, nc, dr = self.kb, self.nc, self.dr
        kb.phase()
        self.common_consts()
        self.norm_setup()
        self.gemm_setup()
        gfm = self.load_gfm("g_mix_bc")
        posT = self.const("posT", [128, 2, 32], F32)
        cmpAB = self.scratch("cmpAB", [2, 4, 2, 128, T], BF16)
        kslcT = self.scratch("kslcT_all", [4, 128, T], BF16)
        vslc = self.scratch("vslc_all", [T, 4, 128], BF16)
        hTs = [kb.tile("hT", [128, 32, 512], BF16) for _ in range(2)]
        stAB = [kb.tile("stAB", [128, 2, 512], BF16) for _ in range(2)]
        stK = [kb.tile("stK", [128, 512], BF16) for _ in range(2)]
        stV = [kb.tile("stV", [128, 4, 128], BF16) for _ in range(2)]
        import os
        NG = int(os.environ.get("KV_GROUPS", T // 512))
        self.make_hT(dr["x_all"][0:512, :], 512, gfm, hTs[0])
        pend_xs = [None]
        for G in range(NG):
            hT = hTs[G % 2]

            def epi(ch, tt, ps, cw, n, G=G):
                kvi, g = ch // 4, ch % 4
                tok0 = G * 512
                if kvi < 2:
                    s = kb.rot("stAB", stAB)
                    pv = ps.t[:, 0:512].rearrange("p (a l) -> p a l", l=16)
                    for ab in range(2):
                        o = s.t[:, ab, :].rearrange("p (a l) -> p a l", l=16)
                        pb = posT.t[:, kvi, ab * 16:(ab + 1) * 16].unsqueeze(1).to_broadcast([128, 32, 16])
                        kb.op("dve", lambda: nc.vector.tensor_tensor(out=o, in0=pv, in1=pb, op=ALU.add), w=[s.b], r=[ps.b, posT.b])
                    kb.dma("pool", [(cmpAB[kvi, g, :, :, tok0:tok0 + 512].rearrange("a p t -> p a t"), s.t[:, :, :])], r=[s.b])
                elif kvi == 2:
                    s = kb.rot("stK", stK)
                    self.evac("act", s.t[:, :], ps.t[:, 0:512], w=[s.b], r=[ps.b])
                    kb.dma("pool", [(kslcT[g, :, tok0:tok0 + 512], s.t[:, :])], r=[s.b])
                else:
                    s = kb.rot("stK", stK)
                    self.evac("act", s.t[:, :], ps.t[:, 0:512], w=[s.b], r=[ps.b])
                    sv = kb.rot("stV", stV)
                    self.fm_to_tm_bf16(s, 512, lambda j: sv.t[:, j, :], sv.b)
                    kb.dma("pool", [(vslc[tok0:tok0 + 512, g, :].rearrange("(j p) d -> p j d", p=128), sv.t[:, :, :])], r=[sv.b])

            def hook(p, G=G):
                if G + 1 < NG:
                    if p % 2 == 0:
                        pend_xs[0] = self.make_hT_prep(dr["x_all"][(G + 1) * 512:(G + 2) * 512, :], p // 2, gfm)
                    else:
                        self.make_hT_xpose(pend_xs[0], p // 2, hTs[(G + 1) % 2])
            self.gemm(dr["w_in_a"], D, 4096, 2048, 512, self.resident_act(hT), epi, hook=hook)

    def phase_own(self):
        kb, nc, dr = self.kb, self.nc, self.dr
        kb.phase()
        self.common_consts()
        self.gemm_setup()
        gfm = self.load_gfm("g_mix_bc")
        qT_d = self.scratch("qT_d", [16, 128, NT], BF16)
        kslcT_o = self.scratch("kslcT_o", [4, 128, NT], BF16)
        vslc_o = self.scratch("vslc_o", [NT, 4, 128], BF16)
        kwinT = self.scratch("kwinT", [4, 128, NE], BF16)
        vwin = self.scratch("vwin", [NE, 4, 128], BF16)
        gates_d = self.scratch("gates_d", [NT, 48], F32)
        gpT = self.scratch("gpT", [D, NT], BF16)
        gnT = self.scratch("gnT", [D, NT], BF16)
        pooledT_d = self.scratch("pooledT_d", [16, 128, NT], BF16)
        uh = kb.tile("uhalo", [128, 16, 16], F32)
        stK = [kb.tile("stK", [128, 512], BF16) for _ in range(2)]
        stV = [kb.tile("stV", [128, 4, 128], BF16) for _ in range(2)]
        stF = [kb.tile("stF", [128, 512], F32) for _ in range(2)]
        stG = [kb.tile("stG", [128, 4, 48], F32) for _ in range(2)]
        hT = kb.tile("hT", [128, 32, NT], BF16)

        def kv_epi(kvi, g, tok0, ps, n):
            s = kb.rot("stK", stK)
            self.evac("act", s.t[:, 0:n], ps.t[:, 0:n], w=[s.b], r=[ps.b])
            if kvi == 2:
                kb.dma("pool", [(kslcT_o[g, :, tok0 - NH:tok0 - NH + n], s.t[:, 0:n])], r=[s.b])
            elif kvi == 4:
                kb.dma("pool", [(kwinT[g, :, tok0:tok0 + n], s.t[:, 0:n])], r=[s.b])
            else:
                sv = kb.rot("stV", stV)
                self.fm_to_tm_bf16(s, n, lambda j: sv.t[:, j, :], sv.b)
                if kvi == 3:
                    dst = vslc_o[tok0 - NH:tok0 - NH + n, g, :]
                else:
                    dst = vwin[tok0:tok0 + n, g, :]
                kb.dma("pool", [(dst.rearrange("(j p) d -> p j d", p=128), sv.t[:, 0:n // 128, :])], r=[sv.b])

        kb.push()
        self.norm_setup()
        self.make_hT(dr["x_ext"][0:NH, :], NH, gfm, hT)
        kb.pop()

        def epi_h_pool(ch, tt, ps, cw, n):
            self.evac("dve", uh.t[:, ch, :], ps.t[:, 496:512], w=[uh.b], r=[ps.b])
        self.gemm(dr["w_in_a"], D, 0, 2048, NH, self.resident_act(hT), epi_h_pool)

        def epi_h_win(ch, tt, ps, cw, n):
            kv_epi(4 + ch // 4, ch % 4, 0, ps, n)
        self.gemm(dr["w_in_a"], D, 4096 + 2048, 1024, NH, self.resident_act(hT), epi_h_win)

        kb.push()
        self.norm_setup()
        self.make_hT(dr["x_ext"][NH:NE, :], NT, gfm, hT)
        kb.pop()

        U = [kb.tile("U", [128, 16 + NT], F32) for _ in range(3)]
        icb = [kb.tile("icb", [128, NT], F32) for _ in range(2)]
        stP = [kb.tile("stP", [128, NT], BF16) for _ in range(2)]
        pend = {}

        def epi_pool(ch, tt, ps, cw, n):
            gi = ch // 4
            if tt == 0:
                u0 = kb.rot("U", U)
                pend[ch] = u0
                kb.op("dve", lambda: nc.vector.tensor_copy(out=u0.t[:, 0:16], in_=uh.t[:, ch, :]), w=[u0.b], r=[uh.b])
            u0 = pend[ch]
            self.evac("act", u0.t[:, 16 + tt * 512:16 + tt * 512 + n], ps.t[:, 0:n], w=[u0.b], r=[ps.b])
            if tt == 1:
                cur = u0
                L = 16 + NT
                sh = 1
                others = [u for u in U if u is not u0]
                for step in range(gi + 1):
                    nxt = others[step % 2]
                    kb.op("dve", lambda: nc.vector.tensor_tensor(out=nxt.t[:, sh:L], in0=cur.t[:, sh:L], in1=cur.t[:, 0:L - sh],
                                                                 op=ALU.add), w=[nxt.b], r=[cur.b])
                    if step >= 1 and cur is not u0:
                        pass
                    cur = nxt
                    sh *= 2
                ic = kb.rot("icb", icb)
                kb.dma("sp", [(ic.t[:, :], dr["invcnt"][gi:gi + 1, :].to_broadcast([128, NT]))], w=[ic.b])
                kb.op("dve", lambda: nc.vector.tensor_tensor(out=cur.t[:, 16:L], in0=cur.t[:, 16:L], in1=ic.t[:, :], op=ALU.mult),
                      w=[cur.b], r=[ic.b])
                sp = kb.rot("stP", stP)
                kb.op("dve", lambda: nc.vector.tensor_tensor(out=sp.t[:, :], in0=cur.t[:, 16:L], in1=u0.t[:, 16:L], op=ALU.subtract),
                      w=[sp.b], r=[cur.b, u0.b])
                kb.dma("pool", [(pooledT_d[ch, :, :], sp.t[:, :])], r=[sp.b])
        self.gemm(dr["w_in_a"], D, 0, 2048, NT, self.resident_act(hT), epi_pool)

        def epi_q(ch, tt, ps, cw, n):
            s = kb.rot("stK", stK)
            kb.op("act", lambda: nc.scalar.activation(out=s.t[:, 0:n], in_=ps.t[:, 0:n], func=AF.Copy, scale=128.0 ** -0.5),
                  w=[s.b], r=[ps.b])
            kb.dma("pool", [(qT_d[ch, :, tt * 512:tt * 512 + n], s.t[:, 0:n])], r=[s.b])
        self.gemm(dr["w_in_a"], D, 2048, 2048, NT, self.resident_act(hT), epi_q)

        def epi_kv(ch, tt, ps, cw, n):
            kv_epi(2 + ch // 4, ch % 4, NH + tt * 512, ps, n)
        self.gemm(dr["w_in_a"], D, 4096 + 1024, 2048, NT, self.resident_act(hT), epi_kv)

        def epi_gates(ch, tt, ps, cw, n):
            s = kb.rot("stF", stF)
            self.evac("dve", s.t[0:48, 0:n], ps.t[0:48, 0:n], w=[s.b], r=[ps.b])
            p2 = kb.rot("psT", self.ps_misc)
            for j in range(4):
                kb.op("pe", lambda: nc.tensor.transpose(out=p2.t[:, j * 48:(j + 1) * 48], in_=s.t[0:48, j * 128:(j + 1) * 128],
                                                        identity=self.ident_f.t[0:48, 0:48]), w=[p2.b], r=[s.b, self.ident_f.b])
            sg = kb.rot("stG", stG)
            kb.op("act", lambda: nc.scalar.activation(out=sg.t[:, :, :], in_=p2.t[:, 0:192].rearrange("p (j c) -> p j c", j=4),
                                                      func=AF.Sigmoid), w=[sg.b], r=[p2.b])
            kb.dma("pool", [(gates_d[tt * 512:(tt + 1) * 512, :].rearrange("(j p) c -> p j c", p=128), sg.t[:, :, :])], r=[sg.b])
        self.gemm(dr["w_in_g"], D, 0, 48, NT, self.resident_act(hT), epi_gates)

        def mk_epi_sig(dst):
            def epi(ch, tt, ps, cw, n):
                s = kb.rot("stK", stK)
                kb.op("act", lambda: nc.scalar.activation(out=s.t[:, 0:n], in_=ps.t[:, 0:n], func=AF.Sigmoid), w=[s.b], r=[ps.b])
                kb.dma("pool", [(dst[ch * 128:(ch + 1) * 128, tt * 512:tt * 512 + n], s.t[:, 0:n])], r=[s.b])
            return epi
        self.gemm(dr["w_in_b"], D, 0, 4096, NT, self.resident_act(hT), mk_epi_sig(gpT))
        self.gemm(dr["w_in_b"], D, 4096, 4096, NT, self.resident_act(hT), mk_epi_sig(gnT))

    def phase_poolg(self):
        kb, nc, dr = self.kb, self.nc, self.dr
        kb.phase()
        self.common_consts()
        self.gemm_setup()
        poT_d = self.scratch("poT_d", [16, 128, NT], BF16)
        pooled = kb.tile("pooled", [128, 16, NT], BF16)
        kb.dma("sp", [(pooled.t[:, :, :], dr["pooledT_d"].rearrange("c p t -> p c t"))], w=[pooled.b])
        psc = self.const("pool_scale", [128, 16], F32)
        stK = [kb.tile("stK", [128, 512], BF16) for _ in range(2)]
        for g in range(4):
            def epi(ch, tt, ps, cw, n, g=g):
                s = kb.rot("stK", stK)
                c16 = g * 4 + ch
                self.evac("act", s.t[:, 0:n], ps.t[:, 0:n], w=[s.b], r=[ps.b], scale=psc.t[:, c16:c16 + 1])
                kb.dma("pool", [(poT_d[c16, :, tt * 512:tt * 512 + n], s.t[:, 0:n])], r=[s.b])
            self.gemm(dr["pool_w"], 512, g * 512, 512, NT, self.resident_act(pooled, kc0=g * 4), epi)

    def phase_cmp(self):
        kb, nc, dr = self.kb, self.nc, self.dr
        kb.phase()
        self.common_consts()
        kcT_d = self.scratch("kcT_d", [4, 128, 512], BF16)
        vc_d = self.scratch("vc_d", [512, 4, 128], BF16)
        wcf = kb.tile("wcf", [128, 32, 128], F32)
        wcb = [kb.tile("wcb", [128, 32, 128], BF16) for _ in range(2)]
        AB = [[kb.tile("cA", [128, T], BF16), kb.tile("cB", [128, T], BF16)] for _ in range(2)]
        stk = [kb.tile("stk", [128, 512], BF16) for _ in range(2)]
        stv = [kb.tile("stv", [128, 128], BF16) for _ in range(2)]
        for s in stk + stv:
            kb.op("dve", lambda: nc.vector.memset(s.t[:, :], 0.0), w=[s.b])
        for kv in range(2):
            wsrc = dr["cmp_w_k" if kv == 0 else "cmp_w_v"]
            kb.dma("sp", [(wcf.t[:, :, :], wsrc.rearrange("(l d) c -> d l c", d=128))], w=[wcf.b])
            wb = wcb[kv]
            kb.op("dve", lambda: nc.vector.tensor_copy(out=wb.t[:, :, :], in_=wcf.t[:, :, :]), w=[wb.b], r=[wcf.b])
            for g in range(4):
                A, B = AB[g % 2]
                kb.dma("sp", [(A.t[:, :], dr["cmpAB"][kv, g, 0, :, :])], w=[A.b])
                kb.dma("sp", [(B.t[:, :], dr["cmpAB"][kv, g, 1, :, :])], w=[B.b])
                Av = A.t[:, :].rearrange("p (n s) -> p n s", s=16)
                Bv = B.t[:, :].rearrange("p (n s) -> p n s", s=16)

                def view(l, n0, n1):
                    if l < 16:
                        return Av[:, n0:n1, l], A.b
                    return Bv[:, n0 + 1:n1 + 1, l - 16], B.b
                if kv == 0:
                    ps = kb.rot("acc", self.ps_acc)
                    for l in range(32):
                        v, vb = view(l, 0, 511)
                        kb.op("pe", lambda: nc.tensor.matmul(ps.t[:, 0:511], lhsT=wb.t[:, l, :], rhs=v, start=(l == 0), stop=(l == 31)),
                              w=[ps.b], r=[wb.b, vb])
                    s = kb.rot("stk", stk)
                    self.evac("act", s.t[:, 0:511], ps.t[:, 0:511], w=[s.b], r=[ps.b])
                    kb.dma("pool", [(kcT_d[g, :, :], s.t[:, :])], r=[s.b])
                else:
                    for ch in range(4):
                        M = 128 if ch < 3 else 127
                        ps = kb.rot("acc", self.ps_acc)
                        for l in range(32):
                            v, vb = view(l, ch * 128, ch * 128 + M)
                            kb.op("pe", lambda: nc.tensor.matmul(ps.t[0:M, 0:128], lhsT=v, rhs=wb.t[:, l, :], start=(l == 0), stop=(l == 31)),
                                  w=[ps.b], r=[wb.b, vb])
                        s = kb.rot("stv", stv)
                        self.evac("dve", s.t[0:M, :], ps.t[0:M, 0:128], w=[s.b], r=[ps.b])
                        kb.dma("pool", [(vc_d[ch * 128:(ch + 1) * 128, g, :], s.t[:, :])], r=[s.b])

    def phase_att(self):
        kb, nc, dr = self.kb, self.nc, self.dr
        kb.phase()
        self.common_consts()
        psS = kb.ps[0:2]
        psS3 = kb.ps[0:3]
        psO = kb.ps[3:7]
        psW = kb.ps[0:6]
        psPW = kb.ps[6:8]
        self.ps_misc = kb.ps[7:8]
        nsaT_d = self.scratch("nsaT_d", [16, 128, NT], BF16)
        ones3 = self.const("ones3", [3, 128], BF16)
        aq = self.const("aq", [3, 16, 512], BF16)
        ehx = self.const("ehx", [128, 56, 128], BF16)
        eox = self.const("eox", [19, 8, 128], BF16)
        selHx = kb.tile("selHx", [128, 4, 512], BF16)
        selOx = kb.tile("selOx", [19, 4, 512], BF16)
        cdiag = self.const("cdiag", [128, 4, 512], BF16)
        kb_o = self.const("kb_o", [128, 16, 2, 8], F32)
        kb_c = self.const("kb_c", [128, 16, 2, 4], F32)
        kb_h = self.const("kb_h", [128, 16, 2, 56], F32)
        cmask = self.const("cmask", [128, 4, 1024], BF16)
        valid_t = self.const("valid_t", [128, 8, 128], F32)
        addt = self.const("addt", [128, 8, 128], F32)
        histcap = self.const("histcap", [128, 1], F32)
        pc = self.const("pc", [128, 16], BF16)
        halo_bias = self.const("halo_bias", [128, 12], F32)
        gates = self.const("gates", [128, 8, 48], F32, src=dr["gates_d"].rearrange("(j p) c -> p j c", p=128))
        ident_b, ident_f = self.ident_b, self.ident_f

        qg = kb.tile("qg", [128, 4, NT], BF16)
        kh = kb.tile("kh", [128, 7168], BF16)
        Vh = kb.tile("Vh", [128, 56, 129], BF16)
        ko = kb.tile("ko", [128, NT], BF16)
        Vo = kb.tile("Vo", [128, 8, 129], BF16)
        kw = kb.tile("kw", [128, NE], BF16)
        Vw = kb.tile("Vw", [128, 12, 129], BF16)
        kc = kb.tile("kc", [128, 512], BF16)
        Rc = kb.tile("Rc", [128, 4, 257], BF16)
        wbg = kb.tile("wbg", [128, 4, 5, 128], F32)
        for V in (Vh, Vo, Vw):
            kb.op("dve", lambda: nc.vector.memset(V.t[:, :, 128:129], 1.0), w=[V.b])
        PTc = [kb.tile("PTc", [128, 4, 512], BF16) for _ in range(2)]
        PTs = [kb.tile("PTs", [128, 512], BF16) for _ in range(3)]
        PTw = [kb.tile("PTw", [128, 5, 128], BF16) for _ in range(3)]
        Sb = [kb.tile("Sb", [128, 5, 128], F32) for _ in range(3)]
        zts = [kb.tile("zt", [128, 4], F32) for _ in range(4)]
        oacc = kb.tile("oacc", [128, 4, 4, 128], F32)
        imp = kb.tile("imp", [128, 4, 128], F32)
        score = [kb.tile("score", [128, 128], F32) for _ in range(2)]
        work = [kb.tile("work", [128, 128], F32) for _ in range(2)]
        m8 = [kb.tile("m8", [128, 16], F32) for _ in range(2)]
        selb = [kb.tile("selb", [128, 128], BF16) for _ in range(2)]
        selT = kb.tile("selT", [128, 512], BF16)
        selTh = kb.tile("selTh", [128, 512], BF16)
        selTo = kb.tile("selTo", [16, 512], BF16)
        stN = [kb.tile("stN", [128, 4, 512], BF16) for _ in range(2)]

        def fac(p2ap, p2b, zcol, s8, gcol, guard):
            zt = kb.rot("zt", zts)
            if guard:
                kb.op("dve", lambda: nc.vector.tensor_scalar(out=zt.t[:, 0:1], in0=p2ap[:, zcol:zcol + 1], scalar1=1e-30, scalar2=None,
                                                             op0=ALU.max), w=[zt.b], r=[p2b])
                kb.op("dve", lambda: nc.vector.reciprocal(out=zt.t[:, 1:2], in_=zt.t[:, 0:1]), w=[zt.b], r=[zt.b])
            else:
                kb.op("dve", lambda: nc.vector.reciprocal(out=zt.t[:, 1:2], in_=p2ap[:, zcol:zcol + 1]), w=[zt.b], r=[p2b])
            kb.op("dve", lambda: nc.vector.tensor_tensor(out=zt.t[:, 2:3], in0=zt.t[:, 1:2], in1=gates.t[:, s8, gcol:gcol + 1], op=ALU.mult),
                  w=[zt.b], r=[zt.b, gates.b])
            return zt

        for g in range(4):
            kb.dma("sp", [(qg.t[:, :, :], dr["qT_d"][4 * g:4 * g + 4, :, :].rearrange("h p t -> p h t"))], w=[qg.b])
            kb.dma("sp", [(kh.t[:, :], dr["kslcT_all"][g, :, 0:7168])], w=[kh.b])
            kb.dma("sp", [(Vh.t[:, :, 0:128], dr["vslc_all"][0:7168, g, :].rearrange("(j p) d -> p j d", p=128))], w=[Vh.b])
            kb.dma("sp", [(ko.t[:, :], dr["kslcT_o"][g, :, :])], w=[ko.b])
            kb.dma("sp", [(Vo.t[:, :, 0:128], dr["vslc_o"][:, g, :].rearrange("(j p) d -> p j d", p=128))], w=[Vo.b])
            kb.dma("sp", [(kw.t[:, :], dr["kwinT"][g, :, :])], w=[kw.b])
            kb.dma("sp", [(Vw.t[:, :, 0:128], dr["vwin"][:, g, :].rearrange("(j p) d -> p j d", p=128))], w=[Vw.b])
            kb.dma("sp", [(kc.t[:, :], dr["kcT_d"][g, :, :])], w=[kc.b])
            kb.dma("sp", [(Rc.t[:, :, 0:128], dr["vc_d"][:, g, :].rearrange("(j p) d -> p j d", p=128)),
                          (Rc.t[:, :, 128:257], dr["ovl1"])], w=[Rc.b])
            kb.dma("sp", [(wbg.t[:, :, :, :], dr["wbias"][:, 4 * g:4 * g + 4, :, :])], w=[wbg.b])
            for qt in range(2):
                q0 = qt * 512
                for hl in range(4):
                    hh = 4 * g + hl
                    pt = kb.rot("PTc", PTc)
                    for ch in range(4):
                        ps = kb.rot("psS", psS)
                        kb.op("pe", lambda: nc.tensor.matmul(ps.t[:, 0:512], lhsT=kc.t[:, ch * 128:(ch + 1) * 128], rhs=qg.t[:, hl, q0:q0 + 512],
                                                             start=True, stop=False), w=[ps.b], r=[kc.b, qg.b])
                        kb.op("pe", lambda: nc.tensor.matmul(ps.t[:, 0:512], lhsT=ones3.t[0:3, :], rhs=aq.t[0:3, hh, :],
                                                             start=False, stop=False), w=[ps.b], r=[ones3.b, aq.b])
                        kb.op("pe", lambda: nc.tensor.matmul(ps.t[:, 0:512], lhsT=ident_b.t[:, :], rhs=cmask.t[:, ch, q0:q0 + 512],
                                                             start=False, stop=True), w=[ps.b], r=[ident_b.b, cmask.b])
                        kb.op("act", lambda: nc.scalar.activation(out=pt.t[:, ch, :], in_=ps.t[:, 0:512], func=AF.Exp,
                                                                  bias=kb_c.t[:, hh, qt, ch:ch + 1]), w=[pt.b], r=[ps.b, kb_c.b])
                    for sub in range(4):
                        s8 = qt * 4 + sub
                        p2 = kb.rot("psT", self.ps_misc)
                        for ch in range(4):
                            kb.op("pe", lambda: nc.tensor.matmul(p2.t[:, 0:257], lhsT=pt.t[:, ch, sub * 128:(sub + 1) * 128], rhs=Rc.t[:, ch, :],
                                                                 start=(ch == 0), stop=(ch == 3)), w=[p2.b], r=[pt.b, Rc.b])
                        zt = fac(p2.t, p2.b, 256, s8, hh * 3 + 0, True)
                        kb.op("dve", lambda: nc.vector.tensor_scalar(out=oacc.t[:, sub, hl, :], in0=p2.t[:, 0:128], scalar1=zt.t[:, 2:3],
                                                                     scalar2=None, op0=ALU.mult), w=[oacc.b], r=[p2.b, zt.b])
                        if hl == 0:
                            kb.op("dve", lambda: nc.vector.tensor_scalar(out=imp.t[:, sub, :], in0=p2.t[:, 128:256], scalar1=zt.t[:, 1:2],
                                                                         scalar2=None, op0=ALU.mult), w=[imp.b], r=[p2.b, zt.b])
                        else:
                            kb.op("dve", lambda: nc.vector.scalar_tensor_tensor(out=imp.t[:, sub, :], in0=p2.t[:, 128:256], scalar=zt.t[:, 1:2],
                                                                                in1=imp.t[:, sub, :], op0=ALU.mult, op1=ALU.add),
                                  w=[imp.b], r=[p2.b, zt.b])
                for sub in range(4):
                    s8 = qt * 4 + sub
                    sc = kb.rot("score", score)
                    wk = kb.rot("work", work)
                    mm = kb.rot("m8", m8)
                    sb = kb.rot("selb", selb)
                    kb.op("dve", lambda: nc.vector.tensor_tensor(out=sc.t[:, :], in0=imp.t[:, sub, :], in1=valid_t.t[:, s8, :], op=ALU.mult),
                          w=[sc.b], r=[imp.b, valid_t.b])
                    kb.op("dve", lambda: nc.vector.tensor_tensor(out=sc.t[:, :], in0=sc.t[:, :], in1=addt.t[:, s8, :], op=ALU.add),
                          w=[sc.b], r=[addt.b])
                    kb.op("dve", lambda: nc.vector.max(out=mm.t[:, 0:8], in_=sc.t[:, :]), w=[mm.b], r=[sc.b])
                    kb.op("dve", lambda: nc.vector.match_replace(out=wk.t[:, :], in_to_replace=mm.t[:, 0:8], in_values=sc.t[:, :], imm_value=-1e30),
                          w=[wk.b], r=[mm.b, sc.b])
                    kb.op("dve", lambda: nc.vector.max(out=mm.t[:, 8:16], in_=wk.t[:, :]), w=[mm.b], r=[wk.b])
                    kb.op("dve", lambda: nc.vector.scalar_tensor_tensor(out=wk.t[:, :], in0=sc.t[:, :], scalar=mm.t[:, 15:16], in1=valid_t.t[:, s8, :],
                                                                        op0=ALU.is_ge, op1=ALU.mult), w=[wk.b], r=[sc.b, mm.b, valid_t.b])
                    kb.op("dve", lambda: nc.vector.tensor_scalar(out=sb.t[:, :], in0=wk.t[:, :], scalar1=BIG, scalar2=-BIG, op0=ALU.mult, op1=ALU.add),
                          w=[sb.b], r=[wk.b])
                    p2 = kb.rot("psT", self.ps_misc)
                    pv = p2.t[:, 0:64].bitcast(BF16)
                    kb.op("pe", lambda: nc.tensor.transpose(out=pv[:, 0:128], in_=sb.t[:, :], identity=ident_b.t[:, :]),
                          w=[p2.b], r=[sb.b, ident_b.b])
                    self.evac("act", selT.t[:, sub * 128:(sub + 1) * 128], pv[:, 0:128], w=[selT.b], r=[p2.b])
                kb.op("dve", lambda: nc.vector.tensor_scalar(out=selTh.t[:, :], in0=selT.t[:, :], scalar1=histcap.t[:, 0:1], scalar2=None, op0=ALU.min),
                      w=[selTh.b], r=[selT.b, histcap.b])
                p2 = kb.rot("psT", self.ps_misc)
                kb.op("pe", lambda: nc.tensor.matmul(p2.t[0:16, 0:512], lhsT=pc.t[:, 0:16], rhs=selT.t[:, :], start=True, stop=True),
                      w=[p2.b], r=[pc.b, selT.b])
                self.evac("act", selTo.t[0:16, :], p2.t[0:16, 0:512], w=[selTo.b], r=[p2.b])
                for hl in range(4):
                    hh = 4 * g + hl
                    kb.op("pool" if hl % 2 else "dve",
                          (lambda: nc.gpsimd.tensor_copy(out=selHx.t[:, hl, :], in_=selTh.t[:, :])) if hl % 2 else
                          (lambda: nc.vector.tensor_copy(out=selHx.t[:, hl, :], in_=selTh.t[:, :])), w=[selHx.b], r=[selTh.b])
                    kb.op("act", lambda: nc.scalar.copy(out=selOx.t[0:16, hl, :], in_=selTo.t[0:16, :]), w=[selOx.b], r=[selTo.b])
                kb.dma("sp", [(selHx.t[112:115, :, :], dr["aq"][:, 4 * g:4 * g + 4, :]),
                              (selOx.t[16:19, :, :], dr["aq"][:, 4 * g:4 * g + 4, :])], w=[selHx.b, selOx.b])
                for hl in range(4):
                    hh = 4 * g + hl
                    chunks = [("h", j) for j in range(56)] + [("o", j) for j in range(4 * qt + 4)]
                    last = len(chunks) - 1
                    pss = {}

                    def scores(idx):
                        kind, j = chunks[idx]
                        ps = kb.rot("psS3", psS3)
                        pss[idx] = ps
                        ksrc = kh if kind == "h" else ko
                        kb.op("pe", lambda: nc.tensor.matmul(ps.t[:, 0:512], lhsT=ksrc.t[:, j * 128:(j + 1) * 128], rhs=qg.t[:, hl, q0:q0 + 512],
                                                             start=True, stop=False), w=[ps.b], r=[ksrc.b, qg.b])
                        if kind == "h":
                            kb.op("pe", lambda: nc.tensor.matmul(ps.t[:, 0:512], lhsT=ehx.t[0:115, j, :], rhs=selHx.t[0:115, hl, :], start=False, stop=True),
                                  w=[ps.b], r=[ehx.b, selHx.b])
                        else:
                            diag = j >= 4 * qt
                            kb.op("pe", lambda: nc.tensor.matmul(ps.t[:, 0:512], lhsT=eox.t[0:19, j, :], rhs=selOx.t[0:19, hl, :], start=False, stop=not diag),
                                  w=[ps.b], r=[eox.b, selOx.b])
                            if diag:
                                kb.op("pe", lambda: nc.tensor.matmul(ps.t[:, 0:512], lhsT=ident_b.t[:, :], rhs=cdiag.t[:, j - 4 * qt, :],
                                                                     start=False, stop=True), w=[ps.b], r=[ident_b.b, cdiag.b])
                    scores(0)
                    scores(1)
                    for idx, (kind, j) in enumerate(chunks):
                        if idx + 2 <= last:
                            scores(idx + 2)
                        ps = pss.pop(idx)
                        if kind == "h":
                            bias, bb, Vt = kb_h.t[:, hh, qt, j:j + 1], kb_h.b, Vh
                        else:
                            bias, bb, Vt = kb_o.t[:, hh, qt, j:j + 1], kb_o.b, Vo
                        pt = kb.rot("PTs", PTs)
                        kb.op("act", lambda: nc.scalar.activation(out=pt.t[:, :], in_=ps.t[:, 0:512], func=AF.Exp, bias=bias),
                              w=[pt.b], r=[ps.b, bb])
                        for sub in range(4):
                            po = psO[sub]
                            kb.op("pe", lambda: nc.tensor.matmul(po.t[:, 0:129], lhsT=pt.t[:, sub * 128:(sub + 1) * 128], rhs=Vt.t[:, j, :],
                                                                 start=(idx == 0), stop=(idx == last)), w=[po.b], r=[pt.b, Vt.b])
                    for sub in range(4):
                        s8 = qt * 4 + sub
                        po = psO[sub]
                        zt = fac(po.t, po.b, 128, s8, hh * 3 + 1, False)
                        kb.op("dve", lambda: nc.vector.scalar_tensor_tensor(out=oacc.t[:, sub, hl, :], in0=po.t[:, 0:128], scalar=zt.t[:, 2:3],
                                                                            in1=oacc.t[:, sub, hl, :], op0=ALU.mult, op1=ALU.add),
                              w=[oacc.b], r=[po.b, zt.b])
                items = [(hl, sub) for hl in range(4) for sub in range(4)]
                wps = {}

                def wscores(ii):
                    hl, sub = items[ii]
                    qb = qt * 4 + sub
                    pa = kb.rot("psW", psW)
                    pb2 = kb.rot("psW", psW)
                    wps[ii] = (pa, pb2)
                    for m in range(5):
                        tgt = pa.t[:, m * 128:(m + 1) * 128] if m < 4 else pb2.t[:, 0:128]
                        tb = pa.b if m < 4 else pb2.b
                        kb.op("pe", lambda: nc.tensor.matmul(tgt, lhsT=kw.t[:, (qb + m) * 128:(qb + m + 1) * 128],
                                                             rhs=qg.t[:, hl, qb * 128:(qb + 1) * 128], start=True, stop=True),
                              w=[tb], r=[kw.b, qg.b])
                wscores(0)
                for ii, (hl, sub) in enumerate(items):
                    hh = 4 * g + hl
                    s8 = qt * 4 + sub
                    qb = s8
                    if ii + 1 < len(items):
                        wscores(ii + 1)
                    pa, pb2 = wps.pop(ii)
                    sbt = kb.rot("Sb", Sb)
                    kb.op("dve", lambda: nc.vector.tensor_tensor(out=sbt.t[:, 0:4, :], in0=pa.t[:, 0:512].rearrange("p (m q) -> p m q", m=4),
                                                                 in1=wbg.t[:, hl, 0:4, :], op=ALU.add), w=[sbt.b], r=[pa.b, wbg.b])
                    kb.op("dve", lambda: nc.vector.tensor_tensor(out=sbt.t[:, 4, :], in0=pb2.t[:, 0:128], in1=wbg.t[:, hl, 4, :], op=ALU.add),
                          w=[sbt.b], r=[pb2.b, wbg.b])
                    ptw = kb.rot("PTw", PTw)
                    for m in range(5):
                        kb.op("act", lambda: nc.scalar.activation(out=ptw.t[:, m, :], in_=sbt.t[:, m, :], func=AF.Exp,
                                                                  bias=halo_bias.t[:, qb + m:qb + m + 1]), w=[ptw.b], r=[sbt.b, halo_bias.b])
                    pw = kb.rot("psPW", psPW)
                    for m in range(5):
                        kb.op("pe", lambda: nc.tensor.matmul(pw.t[:, 0:129], lhsT=ptw.t[:, m, :], rhs=Vw.t[:, qb + m, :],
                                                             start=(m == 0), stop=(m == 4)), w=[pw.b], r=[ptw.b, Vw.b])
                    zt = fac(pw.t, pw.b, 128, s8, hh * 3 + 2, False)
                    kb.op("dve", lambda: nc.vector.scalar_tensor_tensor(out=oacc.t[:, sub, hl, :], in0=pw.t[:, 0:128], scalar=zt.t[:, 2:3],
                                                                        in1=oacc.t[:, sub, hl, :], op0=ALU.mult, op1=ALU.add),
                          w=[oacc.b], r=[pw.b, zt.b])
                sn = kb.rot("stN", stN)
                for hl in range(4):
                    p2 = kb.rot("psT", self.ps_misc)
                    for sub in range(4):
                        kb.op("pe", lambda: nc.tensor.transpose(out=p2.t[:, sub * 128:(sub + 1) * 128], in_=oacc.t[:, sub, hl, :],
                                                                identity=ident_f.t[:, :]), w=[p2.b], r=[oacc.b, ident_f.b])
                    self.evac("act" if hl % 2 else "dve", sn.t[:, hl, :], p2.t[:, 0:512], w=[sn.b], r=[p2.b])
                kb.dma("pool", [(nsaT_d[4 * g:4 * g + 4, :, q0:q0 + 512].rearrange("h p t -> p h t"), sn.t[:, :, :])], r=[sn.b])

    def resid_setup(self):
        kb = self.kb
        self.rs_f = [kb.tile("rsf", [128, 512], F32) for _ in range(2)]
        self.rs_x = [kb.tile("rsx", [128, 4, 128], F32) for _ in range(2)]
        self.rs_o = [kb.tile("rso", [128, 4, 128], F32) for _ in range(2)]

    def resid_epi(self, xsrc, xdst, pre=None):
        kb, nc = self.kb, self.nc

        def epi(ch, tt, ps, cw, n):
            s = kb.rot("rsf", self.rs_f)
            if pre is None:
                self.evac("act", s.t[:, :], ps.t[:, 0:512], w=[s.b], r=[ps.b])
            else:
                pre(ch, tt, ps, s)
            p2 = kb.rot("psT", self.ps_misc)
            for j in range(4):
                kb.op("pe", lambda: nc.tensor.transpose(out=p2.t[:, j * 128:(j + 1) * 128], in_=s.t[:, j * 128:(j + 1) * 128],
                                                        identity=self.ident_f.t[:, :]), w=[p2.b], r=[s.b, self.ident_f.b])
            xt = kb.rot("rsx", self.rs_x)
            kb.dma("sp", [(xt.t[:, :, :], xsrc[tt * 512:(tt + 1) * 512, ch * 128:(ch + 1) * 128].rearrange("(j p) c -> p j c", p=128))], w=[xt.b])
            xo = kb.rot("rso", self.rs_o)
            kb.op("dve", lambda: nc.vector.tensor_tensor(out=xo.t[:, :, :], in0=p2.t[:, 0:512].rearrange("p (j c) -> p j c", j=4),
                                                         in1=xt.t[:, :, :], op=ALU.add), w=[xo.b], r=[p2.b, xt.b])
            kb.dma("pool", [(xdst[tt * 512:(tt + 1) * 512, ch * 128:(ch + 1) * 128].rearrange("(j p) c -> p j c", p=128), xo.t[:, :, :])], r=[xo.b])
        return epi

    def phase_up_out(self):
        kb, nc, dr = self.kb, self.nc, self.dr
        kb.phase()
        self.common_consts()
        self.gemm_setup(3)
        self.resid_setup()
        x1 = self.scratch("x1", [NT, D], F32)
        actT = kb.tile("actT", [128, 16, NT], BF16)
        mres = kb.tile("mres", [128, 32, NT], BF16)
        gt = [kb.tile("gt", [128, 512], BF16) for _ in range(2)]
        tf = [kb.tile("tf", [128, 512], F32) for _ in range(2)]
        kb.dma("sp", [(actT.t[:, :, :], dr["poT_d"].rearrange("c p t -> p c t"))], w=[actT.b])

        def epi1(ch, tt, ps, cw, n):
            gg = kb.rot("gt", gt)
            kb.dma("sp", [(gg.t[:, :], dr["gpT"][ch * 128:(ch + 1) * 128, tt * 512:(tt + 1) * 512])], w=[gg.b])
            kb.op("dve", lambda: nc.vector.tensor_tensor(out=mres.t[:, ch, tt * 512:(tt + 1) * 512], in0=ps.t[:, 0:512], in1=gg.t[:, :], op=ALU.mult),
                  w=[mres.b], r=[ps.b, gg.b])
        self.gemm(dr["w_up_pool"], 2048, 0, D, NT, self.resident_act(actT), epi1)
        kb.dma("sp", [(actT.t[:, :, :], dr["nsaT_d"].rearrange("c p t -> p c t"))], w=[actT.b])

        def epi2(ch, tt, ps, cw, n):
            gg = kb.rot("gt", gt)
            kb.dma("sp", [(gg.t[:, :], dr["gnT"][ch * 128:(ch + 1) * 128, tt * 512:(tt + 1) * 512])], w=[gg.b])
            t = kb.rot("tf", tf)
            kb.op("dve", lambda: nc.vector.tensor_tensor(out=t.t[:, :], in0=ps.t[:, 0:512], in1=gg.t[:, :], op=ALU.mult),
                  w=[t.b], r=[ps.b, gg.b])
            o = mres.t[:, ch, tt * 512:(tt + 1) * 512]
            kb.op("pool", lambda: nc.gpsimd.tensor_tensor(out=o, in0=t.t[:, :], in1=o, op=ALU.add), w=[mres.b], r=[t.b])
        self.gemm(dr["w_up_nsa"], 2048, 0, D, NT, self.resident_act(actT), epi2)
        self.gemm(dr["w_out"], D, 0, D, NT, self.resident_act(mres), self.resid_epi(dr["x_ext"][NH:NE, :], x1))

    def phase_norm(self, xsrc, gname, dst_name):
        kb, nc, dr = self.kb, self.nc, self.dr
        kb.phase()
        self.common_consts()
        self.norm_setup()
        gfm = self.load_gfm(gname + "_bc")
        dst = self.scratch(dst_name, [32, 128, NT], BF16)
        hT = kb.tile("hT", [128, 32, NT], BF16)
        self.make_hT(xsrc, NT, gfm, hT)
        kb.dma("pool", [(dst.rearrange("c p t -> p c t"), hT.t[:, :, :])], r=[hT.b])

    def phase_peer_q(self):
        kb, nc, dr = self.kb, self.nc, self.dr
        kb.phase()
        self.common_consts()
        self.gemm_setup(3)
        qp_d = self.scratch("qp_d", [16, 128, NT], BF16)
        hT = kb.tile("hT", [128, 32, NT], BF16)
        kb.dma("sp", [(hT.t[:, :, :], dr["h2T_d"].rearrange("c p t -> p c t"))], w=[hT.b])
        stK = [kb.tile("stK", [128, 512], BF16) for _ in range(2)]

        def epi(ch, tt, ps, cw, n):
            s = kb.rot("stK", stK)
            self.evac("act", s.t[:, :], ps.t[:, 0:512], w=[s.b], r=[ps.b])
            kb.dma("pool", [(qp_d[ch, :, tt * 512:(tt + 1) * 512], s.t[:, :])], r=[s.b])
        self.gemm(dr["peer_w_q"], D, 0, 2048, NT, self.resident_act(hT), epi)

    def phase_peer_w(self):
        kb, nc, dr = self.kb, self.nc, self.dr
        kb.phase()
        self.common_consts()
        WT = self.scratch("WT_d", [16384, NT], BF16)
        WTv = WT.rearrange("(i j) t -> j i t", j=128)
        qp = kb.tile("qp", [128, 16, NT], BF16)
        kb.dma("sp", [(qp.t[:, :, :], dr["qp_d"].rearrange("c p t -> p c t"))], w=[qp.b])
        kf = self.const("keysT", [128, 2, 128], F32)
        kbf = kb.tile("kbf", [128, 2, 128], BF16)
        kb.op("dve", lambda: nc.vector.tensor_copy(out=kbf.t[:, :, :], in_=kf.t[:, :, :]), w=[kbf.b], r=[kf.b])
        S12s = [kb.tile("S12", [128, 8, 2, 128], F32) for _ in range(2)]
        TK = kb.tile("TK", [128, 8, 2, 16], F32)
        wk = [kb.tile("wk", [128, 128], F32) for _ in range(2)]
        cand = [kb.tile("cand", [128, 16, 16], F32) for _ in range(2)]
        cw2 = [kb.tile("cw2", [128, 256], F32) for _ in range(2)]
        cw3 = [kb.tile("cw3", [128, 256], F32) for _ in range(2)]
        vb = [kb.tile("vb", [128, 24], F32) for _ in range(2)]
        sc = [kb.tile("scl", [128, 16], F32) for _ in range(2)]
        e16 = [kb.tile("e16", [128, 16], F32) for _ in range(2)]
        s1p = [kb.tile("s1p", [128, 8, 128], F32) for _ in range(2)]
        LC = [kb.tile("LC", [128, 8], F32) for _ in range(2)]
        spls = [kb.tile("spl", [128, 8, 128], F32) for _ in range(2)]
        thrs = [kb.tile("thr", [128, 8], F32) for _ in range(2)]
        Et = [kb.tile("Et", [128, 16, 128], F32) for _ in range(3)]
        Mk = [kb.tile("Mk", [128, 16, 128], BF16) for _ in range(16)]
        WTs = [kb.tile("WTs", [128, 16, 128], BF16) for _ in range(2)]
        for t8 in range(8):
            S12 = kb.rot("S12", S12s)
            for hp in range(4):
                ps = kb.rot("acc", self.ps_acc)
                for hq in range(2):
                    h = hp * 2 + hq
                    for s in range(2):
                        kb.op("pe", lambda: nc.tensor.matmul(ps.t[:, (hq * 2 + s) * 128:(hq * 2 + s + 1) * 128],
                                                             lhsT=qp.t[:, 2 * h + s, t8 * 128:(t8 + 1) * 128], rhs=kbf.t[:, s, :],
                                                             start=True, stop=True), w=[ps.b], r=[qp.b, kbf.b])
                self.evac("act" if hp % 2 else "dve", S12.t[:, 2 * hp:2 * hp + 2, :, :].rearrange("p a s n -> p (a s n)"), ps.t[:, 0:512],
                          w=[S12.b], r=[ps.b])
            sp_ = kb.rot("s1p", s1p)
            lc = kb.rot("LC", LC)
            spl = kb.rot("spl", spls)
            thr = kb.rot("thr", thrs)
            for h in range(8):
                for s in range(2):
                    w_ = kb.rot("wk", wk)
                    kb.op("dve", lambda: nc.vector.max(out=TK.t[:, h, s, 0:8], in_=S12.t[:, h, s, :]), w=[TK.b], r=[S12.b])
                    kb.op("dve", lambda: nc.vector.match_replace(out=w_.t[:, :], in_to_replace=TK.t[:, h, s, 0:8], in_values=S12.t[:, h, s, :],
                                                                 imm_value=-1e30), w=[w_.b], r=[TK.b, S12.b])
                    kb.op("dve", lambda: nc.vector.max(out=TK.t[:, h, s, 8:16], in_=w_.t[:, :]), w=[TK.b], r=[w_.b])
                cd = kb.rot("cand", cand)
                kb.op("dve", lambda: nc.vector.tensor_tensor(out=cd.t[:, :, :], in0=TK.t[:, h, 0, :].unsqueeze(2).to_broadcast([128, 16, 16]),
                                                             in1=TK.t[:, h, 1, :].unsqueeze(1).to_broadcast([128, 16, 16]), op=ALU.add),
                      w=[cd.b], r=[TK.b])
                cdf = cd.t[:, :, :].rearrange("p a b -> p (a b)")
                v = kb.rot("vb", vb)
                c2 = kb.rot("cw2", cw2)
                c3 = kb.rot("cw3", cw3)
                kb.op("dve", lambda: nc.vector.max(out=v.t[:, 0:8], in_=cdf), w=[v.b], r=[cd.b])
                kb.op("dve", lambda: nc.vector.match_replace(out=c2.t[:, :], in_to_replace=v.t[:, 0:8], in_values=cdf, imm_value=-1e30),
                      w=[c2.b], r=[v.b, cd.b])
                kb.op("dve", lambda: nc.vector.max(out=v.t[:, 8:16], in_=c2.t[:, :]), w=[v.b], r=[c2.b])
                x = kb.rot("scl", sc)
                e_ = kb.rot("e16", e16)
                kb.op("dve", lambda: nc.vector.tensor_scalar(out=x.t[:, 0:1], in0=v.t[:, 15:16], scalar1=-1e-3, scalar2=None, op0=ALU.add), w=[x.b], r=[v.b])
                kb.op("dve", lambda: nc.vector.tensor_scalar(out=x.t[:, 1:2], in0=v.t[:, 0:1], scalar1=-1.0, scalar2=None, op0=ALU.mult), w=[x.b], r=[v.b])
                kb.op("act", lambda: nc.scalar.activation(out=e_.t[:, :], in_=v.t[:, 0:16], func=AF.Exp, bias=x.t[:, 1:2]),
                      w=[e_.b], r=[v.b, x.b])
                kb.op("dve", lambda: nc.vector.tensor_reduce(out=x.t[:, 2:3], in_=e_.t[:, :], axis=mybir.AxisListType.X, op=ALU.add),
                      w=[x.b], r=[e_.b])
                kb.op("act", lambda: nc.scalar.activation(out=x.t[:, 3:4], in_=x.t[:, 2:3], func=AF.Ln), w=[x.b], r=[x.b])
                kb.op("dve", lambda: nc.vector.tensor_tensor(out=x.t[:, 4:5], in0=x.t[:, 0:1], in1=x.t[:, 1:2], op=ALU.add), w=[x.b], r=[x.b])
                kb.op("dve", lambda: nc.vector.tensor_tensor(out=lc.t[:, h:h + 1], in0=x.t[:, 4:5], in1=x.t[:, 3:4], op=ALU.subtract), w=[lc.b], r=[x.b])
                kb.op("dve", lambda: nc.vector.tensor_scalar(out=sp_.t[:, h, :], in0=S12.t[:, h, 0, :], scalar1=x.t[:, 0:1], scalar2=None,
                                                             op0=ALU.subtract), w=[sp_.b], r=[S12.b, x.b])
                kb.op("dve", lambda: nc.vector.tensor_scalar(out=spl.t[:, h, :], in0=sp_.t[:, h, :], scalar1=lc.t[:, h:h + 1], scalar2=None,
                                                             op0=ALU.add), w=[spl.b], r=[sp_.b, lc.b])
                kb.op("act", lambda: nc.scalar.activation(out=thr.t[:, h:h + 1], in_=lc.t[:, h:h + 1], func=AF.Exp), w=[thr.b], r=[lc.b])
            for ic in range(8):
                accs = [kb.rot("accw", kb.ps) for _ in range(4)]
                mks = []
                for h in range(8):
                    et = kb.rot("Et", Et)
                    mk = kb.rot("Mk", Mk)
                    for i in range(16):
                        ii = ic * 16 + i
                        fn = lambda: nc.scalar.activation(out=et.t[:, i, :], in_=S12.t[:, h, 1, :], func=AF.Exp, bias=spl.t[:, h, ii:ii + 1])
                        if i == 0 or i == 15:
                            kb.op("act", fn, w=[et.b], r=[S12.b, spl.b])
                        else:
                            kb.op("act", fn, r=[S12.b, spl.b])
                    kb.op("dve", lambda: nc.vector.scalar_tensor_tensor(out=mk.t[:, :, :], in0=et.t[:, :, :], scalar=thr.t[:, h:h + 1], in1=et.t[:, :, :],
                                                                        op0=ALU.is_ge, op1=ALU.mult), w=[mk.b], r=[et.b, thr.b])
                    mks.append(mk)
                for i in range(16):
                    a = accs[i // 4]
                    for h in range(8):
                        mk = mks[h]
                        kb.op("pe", lambda: nc.tensor.matmul(a.t[:, (i % 4) * 128:(i % 4 + 1) * 128], lhsT=mk.t[:, i, :], rhs=self.ident_b.t[:, :],
                                                             start=(h == 0), stop=(h == 7)), w=[a.b], r=[mk.b, self.ident_b.b])
                ws = kb.rot("WTs", WTs)
                for b4 in range(4):
                    self.evac("act" if b4 % 2 else "dve", ws.t[:, b4 * 4:(b4 + 1) * 4, :].rearrange("p a t -> p (a t)"), accs[b4].t[:, 0:512],
                              w=[ws.b], r=[accs[b4].b])
                kb.dma("pool", [(WTv[:, ic * 16:(ic + 1) * 16, t8 * 128:(t8 + 1) * 128], ws.t[:, :, :])], r=[ws.b])

    def phase_peer_a(self):
        kb, nc, dr = self.kb, self.nc, self.dr
        kb.phase()
        self.common_consts()
        self.gemm_setup(3)
        coef = self.scratch("coefT_d", [16384, NT], BF16)
        hT = kb.tile("hT", [128, 32, NT], BF16)
        kb.dma("sp", [(hT.t[:, :, :], dr["h2T_d"].rearrange("c p t -> p c t"))], w=[hT.b])
        t1 = [kb.tile("g1", [128, 512], F32) for _ in range(2)]
        t2 = [kb.tile("g2", [128, 512], F32) for _ in range(2)]
        t3 = [kb.tile("g3", [128, 512], F32) for _ in range(2)]
        wt = [kb.tile("gw", [128, 512], BF16) for _ in range(2)]
        cf = [kb.tile("gc", [128, 512], BF16) for _ in range(2)]

        def epi(ch, tt, ps, cw, n):
            a1, a2, a3, w_, c_ = kb.rot("g1", t1), kb.rot("g2", t2), kb.rot("g3", t3), kb.rot("gw", wt), kb.rot("gc", cf)
            kb.dma("sp", [(w_.t[:, :], dr["WT_d"][ch * 128:(ch + 1) * 128, tt * 512:(tt + 1) * 512])], w=[w_.b])
            kb.op("act", lambda: nc.scalar.activation(out=a1.t[:, :], in_=ps.t[:, 0:512], func=AF.Square), w=[a1.b], r=[ps.b])
            kb.op("dve", lambda: nc.vector.tensor_scalar(out=a1.t[:, :], in0=a1.t[:, :], scalar1=0.044715, scalar2=1.0, op0=ALU.mult, op1=ALU.add),
                  w=[a1.b], r=[a1.b])
            kb.op("dve", lambda: nc.vector.tensor_tensor(out=a2.t[:, :], in0=a1.t[:, :], in1=ps.t[:, 0:512], op=ALU.mult), w=[a2.b], r=[a1.b, ps.b])
            kb.op("act", lambda: nc.scalar.activation(out=a3.t[:, :], in_=a2.t[:, :], func=AF.Sigmoid, scale=1.5957691216057308),
                  w=[a3.b], r=[a2.b])
            kb.op("dve", lambda: nc.vector.tensor_tensor(out=a3.t[:, :], in0=a3.t[:, :], in1=ps.t[:, 0:512], op=ALU.mult), w=[a3.b], r=[a3.b, ps.b])
            kb.op("pool", lambda: nc.gpsimd.tensor_tensor(out=c_.t[:, :], in0=a3.t[:, :], in1=w_.t[:, :], op=ALU.mult), w=[c_.b], r=[a3.b, w_.b])
            kb.dma("pool", [(coef[ch * 128:(ch + 1) * 128, tt * 512:(tt + 1) * 512], c_.t[:, :])], r=[c_.b])
        self.gemm(dr["peer_uT"], D, 0, 16384, NT, self.resident_act(hT), epi)

    def phase_peer_o(self):
        kb, nc, dr = self.kb, self.nc, self.dr
        kb.phase()
        self.common_consts()
        self.gemm_setup(3)
        self.resid_setup()
        x2 = self.scratch("x2", [NT, D], F32)
        cT = [kb.tile("cT", [128, 16, NT], BF16) for _ in range(3)]
        cv = dr["coefT_d"].rearrange("(k p) t -> p k t", p=128)

        def act_fn(gi):
            t = kb.rot("cT", cT)
            kb.dma("sp", [(t.t[:, :, :], cv[:, gi * 16:(gi + 1) * 16, :])], w=[t.b])

            def ap(kl, t0, n):
                return t.t[:, kl, t0:t0 + n]
            return ap, t.b
        self.gemm(dr["peer_v"], 16384, 0, D, NT, act_fn, self.resid_epi(dr["x1"], x2))

    def phase_ple(self):
        kb, nc, dr = self.kb, self.nc, self.dr
        kb.phase()
        self.common_consts()
        self.gemm_setup(3)
        self.resid_setup()
        projT = self.scratch("projT_d", [D, NT], F32)
        x3 = self.scratch("x3", [NT, D], F32)
        pf = kb.tile("pf", [128, 2, NT], F32)
        pb = kb.tile("pb", [128, 2, NT], BF16)
        kb.dma("sp", [(pf.t[:, :, :], dr["pT"].rearrange("(k p) t -> p k t", p=128))], w=[pf.b])
        kb.op("dve", lambda: nc.vector.tensor_copy(out=pb.t[:, :, :], in_=pf.t[:, :, :]), w=[pb.b], r=[pf.b])
        stF = [kb.tile("stF", [128, 512], F32) for _ in range(2)]

        def epi_p(ch, tt, ps, cw, n):
            s = kb.rot("stF", stF)
            self.evac("act", s.t[:, :], ps.t[:, 0:512], w=[s.b], r=[ps.b])
            kb.dma("pool", [(projT[ch * 128:(ch + 1) * 128, tt * 512:(tt + 1) * 512], s.t[:, :])], r=[s.b])
        self.gemm(dr["ple_w_proj"], 256, 0, D, NT, self.resident_act(pb), epi_p)
        kb.barrier()
        rT = kb.tile("rT", [128, 32, NT], BF16)
        kb.dma("sp", [(rT.t[:, :, :], dr["rT_d"].rearrange("c p t -> p c t"))], w=[rT.b])
        pj = [kb.tile("pj", [128, 512], F32) for _ in range(2)]
        sg = [kb.tile("sg", [128, 512], F32) for _ in range(2)]

        def pre(ch, tt, ps, s):
            p_ = kb.rot("pj", pj)
            g_ = kb.rot("sg", sg)
            kb.dma("sp", [(p_.t[:, :], projT[ch * 128:(ch + 1) * 128, tt * 512:(tt + 1) * 512])], w=[p_.b])
            kb.op("act", lambda: nc.scalar.activation(out=g_.t[:, :], in_=ps.t[:, 0:512], func=AF.Sigmoid), w=[g_.b], r=[ps.b])
            kb.op("dve", lambda: nc.vector.tensor_tensor(out=s.t[:, :], in0=g_.t[:, :], in1=p_.t[:, :], op=ALU.mult), w=[s.b], r=[g_.b, p_.b])
        self.gemm(dr["ple_w_gate"], D, 0, D, NT, self.resident_act(rT), self.resid_epi(dr["x2"], x3, pre=pre))

    def phase_final(self):
        kb, nc, dr = self.kb, self.nc, self.dr
        kb.phase()
        self.norm_setup()
        gbc = kb.tile("gbc", [128, D], F32)
        kb.dma("sp", [(gbc.t[:, :], dr["g_fin"][0:1, :].to_broadcast([128, D]))], w=[gbc.b])
        for tt in range(NT // 128):
            xt = kb.rot("xb", self.xb)
            s = kb.rot("st", self.st)
            kb.dma("sp", [(xt.t[:, :], dr["x3"][tt * 128:(tt + 1) * 128, :])], w=[xt.b])
            self.row_rstd(xt, s)
            kb.op("dve", lambda: nc.vector.scalar_tensor_tensor(out=xt.t[:, :], in0=xt.t[:, :], scalar=s.t[:, 11:12], in1=gbc.t[:, :],
                                                                op0=ALU.mult, op1=ALU.mult), w=[xt.b], r=[s.b, gbc.b])
            kb.dma("pool", [(dr["y"][tt * 128:(tt + 1) * 128, :], xt.t[:, :])], r=[xt.b])

    PHASES = ["kv_all", "own", "poolg", "cmp", "att", "up_out", "norm2", "peer_q", "peer_w", "peer_a", "peer_o", "norm3", "ple", "final"]

    def build(self):
        dr = self.dr
        for ph in self.PHASES:
            if ph == "norm2":
                self.phase_norm(dr["x1"], "g_ffn", "h2T_d")
            elif ph == "norm3":
                self.phase_norm(dr["x2"], "g_ple", "rT_d")
            else:
                getattr(self, "phase_" + ph)()
            if self.stop_after == ph:
                break
        self.kb.barrier()
        for name, shape, dt in self.debug:
            self.kb.barrier()
            ap = self.dbg_out(name, shape, dt)
            self.kb.dma("sp", [(ap, self.dr[name])])
        self.kb.barrier()
        return self.nc


def prep_shared(inp):
    f = lambda a: np.ascontiguousarray(np.asarray(a, dtype=np.float32))
    sh = {}
    sh["x_all"] = f(inp["x"][0])
    for k, n in (("g_mix", "norm_mix_g"), ("g_ffn", "norm_ffn_g"), ("g_ple", "norm_ple_g")):
        sh[k + "_bc"] = f(inp[n][0]).reshape(1, D)
    sh["g_fin"] = f(inp["norm_final_g"]).reshape(1, D)
    def pan(w, pw=256):
        w = np.asarray(w, np.float32)
        K, N = w.shape
        return np.ascontiguousarray(w.reshape(K // 128, 128, N // pw, pw).transpose(2, 1, 0, 3))
    sh["_pan"] = pan
    w_in = np.asarray(inp["w_in"][0], np.float32)
    sh["w_in_a"] = pan(w_in[:, 0:7168])
    sh["w_in_g"] = pan(w_in[:, 7168:7216], 48)
    sh["w_in_b"] = pan(w_in[:, 7216:])
    pw_ = np.asarray(inp["pool_w"][0], np.float32)
    sh["pool_w"] = np.ascontiguousarray(np.concatenate([pan(pw_[g]) for g in range(4)], axis=0))
    sh["pool_scale"] = f(f(inp["pool_scale"][0]).reshape(16, 128).T)
    sh["posT"] = f(np.stack([f(inp["cmp_pos_k"][0]).T, f(inp["cmp_pos_v"][0]).T], axis=1))
    sh["cmp_w_k"] = f(inp["cmp_w_k"][0])
    sh["cmp_w_v"] = f(inp["cmp_w_v"][0])
    sh["w_up_pool"] = pan(inp["w_up_pool"][0])
    sh["w_up_nsa"] = pan(inp["w_up_nsa"][0])
    sh["w_out"] = pan(inp["w_out"][0])
    sh["peer_w_q"] = pan(inp["peer_w_q"][0])
    sh["keysT"] = f(np.stack([f(inp["peer_keys1"][0]).T, f(inp["peer_keys2"][0]).T], axis=1))
    sh["ple_w_gate"] = pan(inp["ple_w_gate"][0])
    sh["ple_w_proj"] = pan(inp["ple_w_proj"][0])
    sh["_peer_u"] = inp["peer_u"]
    sh["_peer_v"] = inp["peer_v"]
    sh["_p"] = inp["p"]
    sh.update(static_tables())
    return sh


def core_inputs(sh, c, used):
    m = {}
    x = sh["x_all"]
    for name in used:
        if name == "x_ext":
            xe = np.zeros((NE, D), np.float32)
            lo = c * NT - NH
            if lo >= 0:
                xe[:] = x[lo:lo + NE]
            else:
                xe[NH:] = x[0:NT]
            m[name] = xe
        elif name == "pT":
            m[name] = np.ascontiguousarray(np.asarray(sh["_p"][0, 0, c * NT:(c + 1) * NT, :], np.float32).T)
        elif name == "peer_uT":
            if "peer_uT" not in sh:
                sh["peer_uT"] = sh["_pan"](np.asarray(sh["_peer_u"][0], np.float32).T)
            m[name] = sh["peer_uT"]
        elif name == "peer_v":
            if "peer_v" not in sh:
                sh["peer_v"] = sh["_pan"](np.asarray(sh["_peer_v"][0], np.float32))
            m[name] = sh["peer_v"]
        elif name in sh:
            m[name] = sh[name]
        else:
            if ("_core", c) not in sh:
                sh[("_core", c)] = core_tables(c)
            m[name] = sh[("_core", c)][name]
    return m


_CACHE = {}


def kernel(**inputs):
    if "prog" not in _CACHE:
        P = Prog()
        P.build()
        _CACHE["prog"] = P
    P = _CACHE["prog"]
    sh = prep_shared(inputs)
    in_maps = [core_inputs(sh, c, P.used_inputs) for c in range(NCORES)]
    res = run_bass_kernel_spmd(P.nc, in_maps, core_ids=list(range(NCORES)))
    out = np.concatenate([np.asarray(res.results[c]["y"], np.float32) for c in range(NCORES)], axis=0)
    return out.reshape(1, T, D)
```
